# Optimizing a Trainium2 kernel written in Bass

```python
import math
import jax, jax.numpy as jnp
from jax import lax
import numpy as np

D_MODEL = 4096
BATCH = 4
SEQ = 4096
DEPTH = 1

HEAD_DIM = 128
N_HEADS = D_MODEL // HEAD_DIM
HEADS_A = N_HEADS // 2
HEADS_B = N_HEADS - HEADS_A
KV_GROUPS_B = 4
REP_B = HEADS_B // KV_GROUPS_B
WIDTH_A = HEADS_A * HEAD_DIM
WIDTH_B = HEADS_B * HEAD_DIM
KV_WIDTH_B = KV_GROUPS_B * HEAD_DIM
DILATED_CONFIGS = ((128, 1), (512, 4), (2048, 16))
BLK = 128
CMP_LEN = 32
CMP_STRIDE = 16
SLC_BLOCK = 64
N_SELECT = 16
WIN = 512
FORCE_SCORE = 1e6
ROPE_THETA = 500000.0
ROPE_DIM = HEAD_DIM // 4
D_FF = -(-8 * D_MODEL // (3 * 256)) * 256
EPS = 1e-5
IN_SIZES = (WIDTH_A, WIDTH_A, WIDTH_A,
            WIDTH_B,
            KV_WIDTH_B, KV_WIDTH_B,
            KV_WIDTH_B, KV_WIDTH_B,
            KV_WIDTH_B, KV_WIDTH_B,
            3 * HEADS_B)
D_IN = sum(IN_SIZES)
IN_SPLIT_POINTS = tuple(int(v) for v in np.cumsum(IN_SIZES)[:-1])

kernel_name = "hybrid_dilated_nsa_block"


def rmsnorm(x, g):
    xf = x.astype(jnp.float32)
    y = xf * lax.rsqrt(jnp.mean(xf * xf, axis=-1, keepdims=True) + EPS)
    return (y * g.astype(jnp.float32)).astype(x.dtype)


def rope_tables(s):
    inv = ROPE_THETA ** (-jnp.arange(0, ROPE_DIM, 2, dtype=jnp.float32) / ROPE_DIM)
    ang = jnp.arange(s, dtype=jnp.float32)[:, None] * inv[None, :]
    return jnp.cos(ang), jnp.sin(ang)


def apply_rope(x, cos, sin):
    half = ROPE_DIM // 2
    x1 = x[..., :half].astype(jnp.float32)
    x2 = x[..., half:ROPE_DIM].astype(jnp.float32)
    r = jnp.concatenate([x1 * cos - x2 * sin, x1 * sin + x2 * cos], axis=-1).astype(x.dtype)
    return jnp.concatenate([r, x[..., ROPE_DIM:]], axis=-1)


def heads(t, n):
    b, s, _ = t.shape
    return t.reshape(b, s, n, HEAD_DIM).transpose(0, 2, 1, 3)


def banded_attention(q, k, v, max_dist):
    b, g, r, l, dh = q.shape
    nb = l // BLK
    n_prev = -(-max_dist // BLK)
    nk = (n_prev + 1) * BLK
    starts = jnp.arange(nb) * BLK
    kidx = starts[:, None] + jnp.arange(nk)[None, :] - n_prev * BLK
    kcl = jnp.clip(kidx, 0, l - 1)
    kb = jnp.take(k, kcl, axis=2)
    vb = jnp.take(v, kcl, axis=2)
    qpos = starts[:, None] + jnp.arange(BLK)[None, :]
    dist = qpos[:, :, None] - kidx[:, None, :]
    mask = (kidx[:, None, :] >= 0) & (dist >= 0) & (dist <= max_dist)
    qb = q.reshape(b, g, r, nb, BLK, dh)
    s = jnp.einsum('bgrnqd,bgnkd->bgrnqk', qb, kb).astype(jnp.float32) * (HEAD_DIM ** -0.5)
    s = jnp.where(mask, s, -jnp.inf)
    lse = jax.nn.logsumexp(s, axis=-1)
    p = jnp.exp(s - lse[..., None])
    o = jnp.einsum('bgrnqk,bgnkd->bgrnqd', p.astype(v.dtype), vb)
    return o.reshape(b, g, r, l, dh), lse.reshape(b, g, r, l)


def dilated_attention(q, k, v):
    b, h, s, dh = q.shape
    outs, lses = [], []
    for window, dil in DILATED_CONFIGS:
        span = dil * BLK
        sp = -(-s // span) * span
        n_sub = sp // dil

        def strided(t):
            t = jnp.pad(t, ((0, 0), (0, 0), (0, sp - s), (0, 0)))
            return t.reshape(b, h, n_sub, dil, dh).transpose(0, 1, 3, 2, 4).reshape(b, h * dil, n_sub, dh)

        o, lse = banded_attention(strided(q)[:, :, None], strided(k), strided(v), window // dil)
        o = o[:, :, 0].reshape(b, h, dil, n_sub, dh).transpose(0, 1, 3, 2, 4).reshape(b, h, sp, dh)[:, :, :s]
        lse = lse[:, :, 0].reshape(b, h, dil, n_sub).transpose(0, 1, 3, 2).reshape(b, h, sp)[:, :, :s]
        outs.append(o)
        lses.append(lse)
    w = jax.nn.softmax(jnp.stack(lses), axis=0)
    return jnp.einsum('cbhs,cbhsd->bhsd', w.astype(q.dtype), jnp.stack(outs))


def compress(t, pe, w1, w2):
    s = t.shape[2]
    n_c = (s - CMP_LEN) // CMP_STRIDE + 1
    idx = jnp.arange(n_c)[:, None] * CMP_STRIDE + jnp.arange(CMP_LEN)[None, :]
    blocks = jnp.take(t, idx, axis=2) + pe.astype(t.dtype)
    flat = blocks.reshape(blocks.shape[:3] + (CMP_LEN * HEAD_DIM,))
    return jax.nn.gelu(flat @ w1) @ w2


def nsa_attention(q, k_cmp, v_cmp, k_slc, v_slc, k_win, v_win, gates,
                  ck_pe, ck_w1, ck_w2, cv_pe, cv_w1, cv_w2):
    b, g, r, s, dh = q.shape
    scale = HEAD_DIM ** -0.5
    pos = jnp.arange(s)

    kc = compress(k_cmp, ck_pe, ck_w1, ck_w2)
    vc = compress(v_cmp, cv_pe, cv_w1, cv_w2)
    n_c = kc.shape[2]
    c_start = jnp.arange(n_c) * CMP_STRIDE
    c_mask = (c_start + CMP_LEN - 1)[None, :] <= pos[:, None]
    sc = jnp.einsum('bgrsd,bgcd->bgrsc', q, kc).astype(jnp.float32) * scale
    sc = jnp.where(c_mask, sc, -jnp.inf)
    m = jnp.max(sc, axis=-1, keepdims=True)
    m = jnp.where(jnp.isfinite(m), m, 0.0)
    e = jnp.exp(sc - m)
    p_cmp = e / jnp.maximum(jnp.sum(e, axis=-1, keepdims=True), 1e-30)
    o_cmp = jnp.einsum('bgrsc,bgcd->bgrsd', p_cmp.astype(vc.dtype), vc)

    n_s = s // SLC_BLOCK
    s_start = jnp.arange(n_s) * SLC_BLOCK
    span_hits = ((c_start[:, None] < s_start[None, :] + SLC_BLOCK) &
                 (c_start[:, None] + CMP_LEN > s_start[None, :])).astype(jnp.float32)
    imp = jnp.einsum('bgrsc,cj->bgsj', p_cmp, span_hits)
    j = jnp.arange(n_s)[None, :]
    qblk = (pos // SLC_BLOCK)[:, None]
    valid = j * SLC_BLOCK <= pos[:, None]
    forced = (j == 0) | (j == qblk) | (j == qblk - 1)
    score = jnp.where(forced, FORCE_SCORE, jnp.where(valid, imp, -jnp.inf))
    n_top = min(N_SELECT, n_s)
    top_val, top_idx = lax.top_k(score, n_top)
    top_ok = jnp.isfinite(top_val)

    nb = s // BLK
    gather_bg = jax.vmap(jax.vmap(lambda table, idx: table[idx]))
    q_blocks = jnp.moveaxis(q.reshape(b, g, r, nb, BLK, dh), 3, 0)
    idx_blocks = jnp.moveaxis(top_idx.reshape(b, g, nb, BLK, n_top), 2, 0)
    ok_blocks = jnp.moveaxis(top_ok.reshape(b, g, nb, BLK, n_top), 2, 0)
    starts = jnp.arange(nb) * BLK

    def slc_block(args):
        qb, ib, okb, start = args
        tok = (ib[..., None] * SLC_BLOCK + jnp.arange(SLC_BLOCK)).reshape(b, g, BLK, n_top * SLC_BLOCK)
        tok_ok = jnp.broadcast_to(okb[..., None], ib.shape + (SLC_BLOCK,)).reshape(b, g, BLK, n_top * SLC_BLOCK)
        qpos = start + jnp.arange(BLK)
        mask = tok_ok & (tok <= qpos[:, None])
        kg = gather_bg(k_slc, tok)
        vg = gather_bg(v_slc, tok)
        ss = jnp.einsum('bgrqd,bgqtd->bgrqt', qb, kg).astype(jnp.float32) * scale
        ss = jnp.where(mask[:, :, None], ss, -jnp.inf)
        p = jax.nn.softmax(ss, axis=-1)
        return jnp.einsum('bgrqt,bgqtd->bgrqd', p.astype(vg.dtype), vg)

    o_slc = lax.map(slc_block, (q_blocks, idx_blocks, ok_blocks, starts))
    o_slc = jnp.moveaxis(o_slc, 0, 3).reshape(b, g, r, s, dh)

    o_win, _ = banded_attention(q, k_win, v_win, WIN - 1)

    return gates[..., 0:1] * o_cmp + gates[..., 1:2] * o_slc + gates[..., 2:3] * o_win


def setup_inputs(seed: int = 0) -> dict:
    key = jax.random.key(seed)
    ks = jax.random.split(key, 17)
    f32 = jnp.float32
    nrm = lambda k, shape, scale: jax.random.normal(k, shape, f32) * scale
    gain = lambda k, shape: 1.0 + 0.02 * jax.random.normal(k, shape, f32)
    return {
        "x": jax.random.normal(ks[0], (BATCH, SEQ, D_MODEL), f32),
        "norm_attn": gain(ks[1], (DEPTH, D_MODEL)),
        "w_in": nrm(ks[2], (DEPTH, D_MODEL, D_IN), D_MODEL ** -0.5),
        "ck_pe": nrm(ks[3], (DEPTH, CMP_LEN, HEAD_DIM), 0.1),
        "ck_w1": nrm(ks[4], (DEPTH, CMP_LEN * HEAD_DIM, HEAD_DIM), (CMP_LEN * HEAD_DIM) ** -0.5),
        "ck_w2": nrm(ks[5], (DEPTH, HEAD_DIM, HEAD_DIM), HEAD_DIM ** -0.5),
        "cv_pe": nrm(ks[6], (DEPTH, CMP_LEN, HEAD_DIM), 0.1),
        "cv_w1": nrm(ks[7], (DEPTH, CMP_LEN * HEAD_DIM, HEAD_DIM), (CMP_LEN * HEAD_DIM) ** -0.5),
        "cv_w2": nrm(ks[8], (DEPTH, HEAD_DIM, HEAD_DIM), HEAD_DIM ** -0.5),
        "out_norm_a": gain(ks[9], (DEPTH, WIDTH_A)),
        "out_norm_b": gain(ks[10], (DEPTH, WIDTH_B)),
        "w_out": nrm(ks[11], (DEPTH, D_MODEL, D_MODEL), D_MODEL ** -0.5),
        "norm_ffn": gain(ks[12], (DEPTH, D_MODEL)),
        "w_gate": nrm(ks[13], (DEPTH, D_MODEL, D_FF), D_MODEL ** -0.5),
        "w_up": nrm(ks[14], (DEPTH, D_MODEL, D_FF), D_MODEL ** -0.5),
        "w_down": nrm(ks[15], (DEPTH, D_FF, D_MODEL), D_FF ** -0.5),
        "norm_final": gain(ks[16], (D_MODEL,)),
    }


def reference(x, norm_attn, w_in, ck_pe, ck_w1, ck_w2, cv_pe, cv_w1, cv_w2,
              out_norm_a, out_norm_b, w_out, norm_ffn, w_gate, w_up, w_down, norm_final):
    b, s, _ = x.shape
    cos, sin = rope_tables(s)
    for l in range(DEPTH):
        h = rmsnorm(x, norm_attn[l])
        proj = h @ w_in[l]
        (qa, ka, va, qb, kbc, vbc, kbs, vbs, kbw, vbw, gl) = jnp.split(proj, IN_SPLIT_POINTS, axis=-1)

        qa = apply_rope(heads(qa, HEADS_A), cos, sin)
        ka = apply_rope(heads(ka, HEADS_A), cos, sin)
        o_a = dilated_attention(qa, ka, heads(va, HEADS_A))

        q_b = apply_rope(heads(qb, HEADS_B), cos, sin).reshape(b, KV_GROUPS_B, REP_B, s, HEAD_DIM)
        k_c = apply_rope(heads(kbc, KV_GROUPS_B), cos, sin)
        k_s = apply_rope(heads(kbs, KV_GROUPS_B), cos, sin)
        k_w = apply_rope(heads(kbw, KV_GROUPS_B), cos, sin)
        gates = jax.nn.sigmoid(gl.reshape(b, s, KV_GROUPS_B, REP_B, 3).transpose(0, 2, 3, 1, 4))
        o_b = nsa_attention(q_b, k_c, heads(vbc, KV_GROUPS_B), k_s, heads(vbs, KV_GROUPS_B),
                            k_w, heads(vbw, KV_GROUPS_B), gates,
                            ck_pe[l], ck_w1[l], ck_w2[l], cv_pe[l], cv_w1[l], cv_w2[l])

        o_a = o_a.transpose(0, 2, 1, 3).reshape(b, s, WIDTH_A)
        o_b = o_b.reshape(b, HEADS_B, s, HEAD_DIM).transpose(0, 2, 1, 3).reshape(b, s, WIDTH_B)
        mixed = jnp.concatenate([rmsnorm(o_a, out_norm_a[l]), rmsnorm(o_b, out_norm_b[l])], axis=-1)
        x = x + mixed @ w_out[l]

        h = rmsnorm(x, norm_ffn[l])
        x = x + (jax.nn.silu(h @ w_gate[l]) * (h @ w_up[l])) @ w_down[l]
    return rmsnorm(x, norm_final)
```

```python
import os
import numpy as np
import ml_dtypes
from contextlib import ExitStack
import concourse.bass as bass
import concourse.mybir as mybir
from concourse.bass_utils import run_bass_kernel_spmd

F32 = mybir.dt.float32
BF16 = mybir.dt.bfloat16
AF = mybir.ActivationFunctionType
ALU = mybir.AluOpType
AX = mybir.AxisListType
NPBF = ml_dtypes.bfloat16

DM = 4096
SEQ = 4096
OWN = 2048
CTX = 4096
DFF = 11008
DIN = 11312
EPS = 1e-5
SCALE = 128 ** -0.5
NTT = 4
TT = 512


class Sched:
    def __init__(self, nc, stack):
        self.nc = nc
        self.stack = stack
        self.eng = {"pe": nc.tensor, "dve": nc.vector, "act": nc.scalar, "pool": nc.gpsimd, "sp": nc.sync}
        self.sem = {}
        self.cnt = {}
        self.known = {e: {} for e in self.eng}
        self.lastw = {}
        self.readers = {}
        self.nwaits = 0

    def _key(self, key):
        if key not in self.sem:
            self.sem[key] = self.stack.enter_context(self.nc.semaphore("s_" + key))
            self.cnt[key] = 0
        return key

    def _collect(self, e, reads, writes):
        deps = {}

        def need(k, v, raw):
            if k == e and not raw:
                return
            if deps.get(k, 0) < v:
                deps[k] = v

        for r in reads:
            w = self.lastw.get(r)
            if w:
                need(w[0], w[1], True)
        for r in writes:
            w = self.lastw.get(r)
            if w:
                need(w[0], w[1], False)
            for k, v in self.readers.get(r, {}).items():
                need(k, v, False)
        for k, v in deps.items():
            if self.known[e].get(k, 0) < v:
                self.eng[e].wait_ge(self.sem[k], v)
                self.known[e][k] = v
                self.nwaits += 1

    def _record(self, key, reads, writes):
        v = self.cnt[key]
        for r in reads:
            self.readers.setdefault(r, {})[key] = v
        for w in writes:
            self.lastw[w] = (key, v)
            self.readers[w] = {}

    def op(self, e, fn, reads=(), writes=()):
        self._key(e)
        self._collect(e, reads, writes)
        ins = fn()
        self.cnt[e] += 1
        ins.then_inc(self.sem[e], 1)
        self._record(e, reads, writes)

    def dma(self, q, key, out, in_, reads=(), writes=()):
        key = self._key("d_" + key)
        self._collect(q, reads, writes)
        ins = self.eng[q].dma_start(out=out, in_=in_)
        self.cnt[key] += 16
        ins.then_inc(self.sem[key], 16)
        self._record(key, reads, writes)

    def barrier(self, engines=("pe", "dve", "act", "pool", "sp")):
        for e in engines:
            for k, v in self.cnt.items():
                if k == e:
                    continue
                if self.known[e].get(k, 0) < v:
                    self.eng[e].wait_ge(self.sem[k], v)
                    self.known[e][k] = v

    def final_wait(self, e="sp"):
        for k, v in self.cnt.items():
            if k.startswith("d_") and self.known[e].get(k, 0) < v:
                self.eng[e].wait_ge(self.sem[k], v)
                self.known[e][k] = v


def dap(t, offset, pattern):
    return bass.AP(t, offset, [list(p) for p in pattern])


C_QA, C_KA, C_VA, C_QB = 0, 2048, 4096, 6144
C_KBC, C_VBC, C_KBS, C_VBS, C_KBW, C_VBW, C_GL = 8192, 8704, 9216, 9728, 10240, 10752, 11264
KT_KA, KT_KBC, KT_KBS, KT_KBW, KT_VBC = 0, 16, 20, 24, 28
V_VA, V_VBS, V_VBW = 0, 2048, 2560


def build_program(debug=False, stop_after=None, tiles=range(8), tiles_d=range(4), tiles_c=range(4), skip_attn=False):
    nc = bass.Bass("TRN2", target_bir_lowering=False)
    stack = ExitStack()
    S = Sched(nc, stack)
    dk = "ExternalOutput" if debug else "Internal"

    def din(name, shape, dt=F32):
        return nc.dram_tensor(name, list(shape), dt, kind="ExternalInput")

    xc = din("xc", [CTX, DM])
    w_in = din("w_in", [DM, DIN])
    w_out = din("w_out", [DM, DM])
    w_gate = din("w_gate", [DM, DFF])
    w_up = din("w_up", [DM, DFF])
    w_down = din("w_down", [DFF, DM])
    cw1 = [din("ck_w1", [4096, 128]), din("cv_w1", [4096, 128])]
    cw2 = [din("ck_w2", [128, 128]), din("cv_w2", [128, 128])]
    cpeT = [din("ck_peT", [128, 32]), din("cv_peT", [128, 32])]
    g_attn = din("g_attn", [1, DM])
    g_ffn = din("g_ffn", [1, DM])
    g_fin = din("g_fin", [1, DM])
    g_outT = din("g_outT", [128, 32])
    ropeC = din("ropeC", [128, CTX])
    ropeS = din("ropeS", [128, CTX])
    mA_d = din("mA", [128, 3072], BF16)
    mW_d = din("mW", [128, 1408], BF16)
    mC_d = din("mC", [128, 1024], BF16)
    cmask_d = din("cmaskT", [128, 2, OWN], BF16)
    span_d = din("span", [128, 2, 64], BF16)
    VM_d = din("VM", [128, 16, 64])
    FB_d = din("FB", [128, 16, 64])
    eexp_d = din("eexp", [128, 32, 128], BF16)
    kval_d = din("kval", [128, 2, 128], BF16)
    ident_d = din("ident", [128, 128], BF16)
    rm_d = din("rm", [128, 128], BF16)
    y = nc.dram_tensor("y", [OWN, DM], F32, kind="ExternalOutput")
    qT = nc.dram_tensor("qT", [32, 128, OWN], BF16, kind=dk)
    kT = nc.dram_tensor("kT", [32, 128, CTX], BF16, kind=dk)
    vv = nc.dram_tensor("vv", [CTX, 3072], BF16, kind=dk)
    gTd = nc.dram_tensor("gTd", [48, OWN], F32, kind=dk)
    kcT_d = nc.dram_tensor("kcT", [4, 128, 256], BF16, kind=dk)
    vc_d = nc.dram_tensor("vc", [4, 256, 128], BF16, kind=dk)
    oT = nc.dram_tensor("oT", [32, 128, OWN], BF16, kind=dk)
    x1d = nc.dram_tensor("x1d", [OWN, DM], F32, kind=dk)

    uid = [0]

    def sb(name, shape, dt, st=None):
        uid[0] += 1
        return (st or stack).enter_context(nc.sbuf_tensor("sb%d_%s" % (uid[0], name), list(shape), dt))

    def ps(name, shape, dt, st=None):
        uid[0] += 1
        return (st or stack).enter_context(nc.psum_tensor("ps%d_%s" % (uid[0], name), list(shape), dt))

    ident = sb("ident", [128, 128], BF16)
    rm = sb("rm", [128, 128], BF16)
    ones = sb("ones", [128, 128], BF16)
    S.dma("sp", "c0", ident[:], ident_d.ap(), writes=["ident"])
    S.dma("sp", "c1", rm[:], rm_d.ap(), writes=["rm"])
    S.op("dve", lambda: nc.vector.memset(ones[:], 1.0), writes=["ones"])
    epsb = sb("epsb", [128, 1], F32)
    S.op("dve", lambda: nc.vector.memset(epsb[:], EPS), writes=["epsb"])

    def norm_transpose(st_name, xin, gain_bc, xs, hT, blk, tp, ssq, rstd, tpi):
        S.op("act", lambda: nc.scalar.activation(out=xs[:], in_=xin, func=AF.Square, scale=1.0 / 64.0, accum_out=ssq[:]),
             reads=[st_name], writes=["xs", "ssq"])
        S.op("act", lambda: nc.scalar.activation(out=ssq[:], in_=ssq[:], func=AF.Sqrt, bias=epsb[:]),
             reads=["ssq", "epsb"], writes=["ssq"])
        S.op("dve", lambda: nc.vector.reciprocal(out=rstd[:], in_=ssq[:]), reads=["ssq"], writes=["rstd"])
        S.op("dve", lambda: nc.vector.scalar_tensor_tensor(out=xs[:], in0=xin, scalar=rstd[:, 0:1], in1=gain_bc[:],
                                                           op0=ALU.mult, op1=ALU.mult),
             reads=[st_name, "rstd", "gain"], writes=["xs"])
        for g8 in range(4):
            t = tp[tpi[0] % 2]
            tn = "tp%d" % (tpi[0] % 2)
            tpi[0] += 1
            for j in range(8):
                kc = g8 * 8 + j
                S.op("pe", lambda kc=kc, j=j, t=t: nc.tensor.transpose(out=t[:, j * 128:(j + 1) * 128],
                                                                     in_=xs[:, kc * 128:(kc + 1) * 128],
                                                                     identity=ident[:]),
                     reads=["xs", "ident"], writes=[tn])
            dst = hT[:, g8 * 8:(g8 + 1) * 8, blk * 128:(blk + 1) * 128]
            src = t[:].rearrange("p (a b) -> p a b", a=8)
            if g8 % 2 == 0:
                S.op("act", lambda dst=dst, src=src: nc.scalar.copy(out=dst, in_=src), reads=[tn], writes=["hT"])
            else:
                S.op("dve", lambda dst=dst, src=src: nc.vector.tensor_copy(out=dst, in_=src), reads=[tn], writes=["hT"])

    with ExitStack() as pa:
        gain = sb("gainA", [128, DM], F32, pa)
        xblk = [sb("xblk%d" % i, [128, DM], F32, pa) for i in range(2)]
        xs = sb("xsA", [128, DM], BF16, pa)
        hT = sb("hTA", [128, 32, TT], BF16, pa)
        Wt = [sb("WtA%d" % i, [128, 32, 512], BF16, pa) for i in range(2)]
        rC = sb("rC", [128, TT], F32, pa)
        rS = sb("rS", [128, TT], F32, pa)
        qsb = [sb("qsb%d" % i, [128, TT], BF16, pa) for i in range(4)]
        t1 = [sb("t1_%d" % i, [128, TT], F32, pa) for i in range(2)]
        t2 = [sb("t2_%d" % i, [128, TT], F32, pa) for i in range(2)]
        gsb = sb("gsb", [128, TT], F32, pa)
        ssq = sb("ssqA", [128, 1], F32, pa)
        rstd = sb("rstdA", [128, 1], F32, pa)
        tp = [ps("tpA%d" % i, [128, 1024], BF16, pa) for i in range(2)]
        acc = [ps("accA%d" % i, [128, 512], F32, pa) for i in range(4)]
        ps2 = ps("ps2A", [128, 512], F32, pa)

        S.dma("sp", "gain", gain[:], dap(g_attn, 0, [[0, 128], [1, DM]]), writes=["gain"])
        tpi = [0]
        wi = [0]
        acci = [0]
        qi = [0]
        ti = [0]
        blocks = []
        for j in range(4):
            blocks.append((C_QA + 512 * j, 512, "q", 4 * j, True, True))
        for j in range(4):
            blocks.append((C_KA + 512 * j, 512, "k", KT_KA + 4 * j, True, False))
        for j in range(4):
            blocks.append((C_VA + 512 * j, 512, "v", V_VA + 512 * j, False, False))
        for j in range(4):
            blocks.append((C_QB + 512 * j, 512, "q", 16 + 4 * j, True, True))
        blocks.append((C_KBC, 512, "k", KT_KBC, True, False))
        blocks.append((C_VBC, 512, "k", KT_VBC, False, False))
        blocks.append((C_KBS, 512, "k", KT_KBS, True, False))
        blocks.append((C_VBS, 512, "v", V_VBS, False, False))
        blocks.append((C_KBW, 512, "k", KT_KBW, True, False))
        blocks.append((C_VBW, 512, "v", V_VBW, False, False))
        blocks.append((C_GL, 48, "g", 0, False, True))

        for tile in tiles:
            own = tile >= 4
            tok0 = tile * TT
            for blk in range(4):
                xb = xblk[(tile * 4 + blk) % 2]
                xn = "xblk%d" % ((tile * 4 + blk) % 2)
                r0 = tok0 + blk * 128
                S.dma("sp", xn, xb[:], xc.ap()[r0:r0 + 128, :], writes=[xn])
                norm_transpose(xn, xb[:], gain, xs, hT, blk, tp, ssq, rstd, tpi)
            S.dma("sp", "rC", rC[:], ropeC.ap()[:, tok0:tok0 + TT], writes=["rC"])
            S.dma("sp", "rS", rS[:], ropeS.ap()[:, tok0:tok0 + TT], writes=["rS"])

            def post_fm(bank, bn, kind, dest, rope, hh):
                if os.environ.get("KNOPOST"):
                    return
                if os.environ.get("KNOROPE"):
                    rope = False
                s = qi[0] % 4
                qi[0] += 1
                q = qsb[s]
                qn = "qsb%d" % s
                S.op("act", lambda: nc.scalar.copy(out=q[:], in_=bank[:]), reads=[bn], writes=[qn])
                if rope:
                    u = ti[0] % 2
                    ti[0] += 1
                    S.op("pe", lambda: nc.tensor.matmul(ps2[:], lhsT=rm[:], rhs=q[:], start=True, stop=True),
                         reads=[qn, "rm"], writes=["ps2"])
                    S.op("dve", lambda: nc.vector.tensor_tensor(out=t1[u][:], in0=bank[:], in1=rC[:], op=ALU.mult),
                         reads=[bn, qn, "rC"], writes=["t1_%d" % u])
                    S.op("dve", lambda: nc.vector.tensor_tensor(out=t2[u][:], in0=ps2[:], in1=rS[:], op=ALU.mult),
                         reads=["ps2", "rS"], writes=["t2_%d" % u])
                    S.op("dve", lambda: nc.vector.tensor_tensor(out=q[:], in0=t1[u][:], in1=t2[u][:], op=ALU.add),
                         reads=["t1_%d" % u, "t2_%d" % u], writes=[qn])
                if kind == "q":
                    dst = qT.ap()[dest + hh, :, tok0 - OWN:tok0 - OWN + TT]
                else:
                    dst = kT.ap()[dest + hh, :, tok0:tok0 + TT]
                S.dma("sp", qn, dst, q[:], reads=[qn], writes=["dram_qk"])

            def post_v(bank, bn, vcol, tb):
                s = qi[0] % 4
                qi[0] += 1
                q = qsb[s]
                qn = "qsb%d" % s
                S.op("act", lambda: nc.scalar.copy(out=q[:], in_=bank[:]), reads=[bn], writes=[qn])
                r0 = tok0 + tb * 128
                S.dma("sp", qn, vv.ap()[r0:r0 + 128, vcol:vcol + 512], q[:], reads=[qn], writes=["dram_v"])

            def post_g(bank, bn):
                S.op("act", lambda: nc.scalar.activation(out=gsb[:], in_=bank[:], func=AF.Sigmoid),
                     reads=[bn], writes=["gsb"])
                S.dma("sp", "gsb", gTd.ap()[:, tok0 - OWN:tok0 - OWN + TT], gsb[0:48, :], reads=["gsb"], writes=["dram_g"])

            pending = None
            KD = int(os.environ.get("KDBG", "0"))
            for bi, (c0, ncol, kind, dest, rope, own_only) in enumerate(blocks):
                if own_only and not own:
                    continue
                if KD == 1 or (KD >= 2 and bi not in (0, 8, 22)[:KD - 1]):
                    continue
                w = Wt[wi[0] % 2]
                wn = "WtA%d" % (wi[0] % 2)
                wi[0] += 1
                wsrc = w_in.ap()[:, c0:c0 + ncol].rearrange("(kc p) c -> p kc c", p=128)
                for k4 in range(8):
                    S.dma("pool", wn, w[:, 4 * k4:4 * k4 + 4, 0:ncol], wsrc[:, 4 * k4:4 * k4 + 4, :], writes=[wn])
                nunits = 1 if kind == "g" else 4
                for un in range(nunits):
                    b = acci[0] % 4
                    acci[0] += 1
                    bank = acc[b]
                    bn = "accA%d" % b
                    for kc in range(32):
                        if kind in ("q", "k"):
                            S.op("pe", lambda kc=kc, un=un: nc.tensor.matmul(bank[:], lhsT=w[:, kc, un * 128:(un + 1) * 128],
                                                                        rhs=hT[:, kc, :], start=(kc == 0), stop=(kc == 31)),
                                 reads=[wn, "hT"], writes=[bn])
                        elif kind == "v":
                            S.op("pe", lambda kc=kc, un=un: nc.tensor.matmul(bank[:], lhsT=hT[:, kc, un * 128:(un + 1) * 128],
                                                                        rhs=w[:, kc, :], start=(kc == 0), stop=(kc == 31)),
                                 reads=[wn, "hT"], writes=[bn])
                        else:
                            S.op("pe", lambda kc=kc: nc.tensor.matmul(bank[:], lhsT=w[:, kc, 0:128],
                                                                 rhs=hT[:, kc, :], start=(kc == 0), stop=(kc == 31)),
                                 reads=[wn, "hT"], writes=[bn])
                    if pending is not None:
                        pending()
                    if kind in ("q", "k"):
                        pending = (lambda bank=bank, bn=bn, kind=kind, dest=dest, rope=rope, un=un:
                                   post_fm(bank, bn, kind, dest, rope, un))
                    elif kind == "v":
                        pending = (lambda bank=bank, bn=bn, dest=dest, un=un: post_v(bank, bn, dest, un))
                    else:
                        pending = (lambda bank=bank, bn=bn: post_g(bank, bn))
            if pending is not None:
                pending()
        S.barrier()
    if stop_after == "A":
        return finish(nc, S, stack, y)


    if not skip_attn:
        attention_phases(nc, S, stack, sb, ps, dict(
            qT=qT, kT=kT, vv=vv, gTd=gTd, oT=oT, cw1=cw1, cw2=cw2, cpeT=cpeT, mA=mA_d, mW=mW_d, mC=mC_d,
            cmask=cmask_d, span=span_d, VM=VM_d, FB=FB_d, eexp=eexp_d, kval=kval_d, ones=ones, ident=ident),
            tiles_c)
    else:
        with ExitStack() as pz:
            zt = sb("zeroT", [128, OWN], BF16, pz)
            S.op("dve", lambda: nc.vector.memset(zt[:], 0.0), writes=["zt"])
            for h in range(32):
                S.dma("sp", "zt", oT.ap()[h, :, :], zt[:], reads=["zt"], writes=["dram_oT"])
            S.barrier()
    gout = sb("gout", [128, 32], F32)
    S.dma("sp", "c2", gout[:], g_outT.ap(), writes=["gout"])
    for t in tiles_d:
        q0 = t * TT
        with ExitStack() as pt:
          h2T = sb("h2T", [128, 32, TT], BF16, pt)
          with ExitStack() as pdx:
           x1 = sb("x1D", [128, 4, DM], F32, pdx)
           with ExitStack() as pd:
            OT = sb("OT", [128, 32, TT], BF16, pd)
            sq = [sb("sqD%d" % i, [128, 8, TT], BF16, pd) for i in range(2)]
            rs = [sb("rsD%d" % i, [128, TT], F32, pd) for i in range(2)]
            Wo = [sb("WoD%d" % i, [128, 32, 256], BF16, pd) for i in range(2)]
            xr = [sb("xrD%d" % i, [128, 4, 256], F32, pd) for i in range(2)]
            ssp = [ps("sspD%d" % i, [128, TT], F32, pd) for i in range(2)]
            acc = [ps("accD%d" % i, [128, 256], F32, pd) for i in range(4)]
            S.dma("sp", "OT", OT[:], oT.ap()[:, :, q0:q0 + TT].rearrange("h d t -> d h t"), reads=["dram_oT"], writes=["OT"])
            for g in range(2):
                for i8 in range(2):
                    u = (g * 2 + i8) % 2
                    h0 = g * 16 + i8 * 8
                    S.op("act", lambda u=u, h0=h0: nc.scalar.activation(out=sq[u][:], in_=OT[:, h0:h0 + 8, :], func=AF.Square),
                         reads=["OT"], writes=["sq%d" % u])
                    for j in range(8):
                        S.op("pe", lambda u=u, j=j, g=g, i8=i8: nc.tensor.matmul(ssp[g][:], lhsT=ones[:], rhs=sq[u][:, j, :],
                                                                          start=(i8 == 0 and j == 0), stop=(i8 == 1 and j == 7)),
                             reads=["sq%d" % u, "ones"], writes=["ssp%d" % g])
                S.op("act", lambda g=g: nc.scalar.activation(out=rs[g][:], in_=ssp[g][:], func=AF.Sqrt, scale=1.0 / 2048.0, bias=epsb[:]),
                     reads=["ssp%d" % g, "epsb"], writes=["rs%d" % g])
                S.op("dve", lambda g=g: nc.vector.reciprocal(out=rs[g][:], in_=rs[g][:]), reads=["rs%d" % g], writes=["rs%d" % g])
            for h in range(32):
                S.op("dve", lambda h=h: nc.vector.scalar_tensor_tensor(out=OT[:, h, :], in0=OT[:, h, :], scalar=gout[:, h:h + 1],
                                                                  in1=rs[h // 16][:], op0=ALU.mult, op1=ALU.mult),
                     reads=["OT", "gout", "rs%d" % (h // 16)], writes=["OT"])
            for cb in range(16):
                w = Wo[cb % 2]
                wn = "WoD%d" % (cb % 2)
                c0 = cb * 256
                wsrc = w_out.ap()[:, c0:c0 + 256].rearrange("(h p) c -> p h c", p=128)
                for k4 in range(4):
                    S.dma("pool", wn, w[:, 8 * k4:8 * k4 + 8, :], wsrc[:, 8 * k4:8 * k4 + 8, :], writes=[wn])
                xrt = xr[cb % 2]
                xn = "xrD%d" % (cb % 2)
                S.dma("sp", xn, xrt[:], xc.ap()[OWN + q0:OWN + q0 + TT, c0:c0 + 256].rearrange("(b p) c -> p b c", p=128),
                      writes=[xn])
                for tb in range(4):
                    for h in range(32):
                        S.op("pe", lambda tb=tb, h=h: nc.tensor.matmul(acc[tb][:], lhsT=OT[:, h, tb * 128:(tb + 1) * 128], rhs=w[:, h, :],
                                                                  start=(h == 0), stop=(h == 31)),
                             reads=["OT", wn], writes=["accD%d" % tb])
                    S.op("dve", lambda tb=tb: nc.vector.tensor_tensor(out=x1[:, tb, c0:c0 + 256], in0=acc[tb][:], in1=xrt[:, tb, :], op=ALU.add),
                         reads=["accD%d" % tb, xn], writes=["x1"])
            S.dma("sp", "x1o", x1d.ap()[q0:q0 + TT, :].rearrange("(b p) c -> p b c", p=128), x1[:], reads=["x1"], writes=["dram_x1"])
            S.barrier()
           if True:
            with ExitStack() as pd3:
                gainF = sb("gainF", [128, DM], F32, pd3)
                xs = sb("xsD", [128, DM], BF16, pd3)
                ssq = sb("ssqD", [128, 1], F32, pd3)
                rstd = sb("rstdD", [128, 1], F32, pd3)
                tp = [ps("tpD%d" % i, [128, 1024], BF16, pd3) for i in range(2)]
                S.dma("sp", "gain", gainF[:], dap(g_ffn, 0, [[0, 128], [1, DM]]), writes=["gain"])
                tpi = [0]
                for tb in range(4):
                    norm_transpose("x1", x1[:, tb, :], gainF, xs, h2T, tb, tp, ssq, rstd, tpi)
                S.barrier()
          ffn_tile(nc, S, stack, sb, ps, t, q0, h2T, w_gate, w_up, w_down, x1d, g_fin, y, epsb)
          S.barrier()
    return finish(nc, S, stack, y)


def attention_phases(nc, S, stack, sb, ps, T, tiles_c):
    qT, kT, vv, gTd, oT = T["qT"], T["kT"], T["vv"], T["gTd"], T["oT"]
    ones, ident = T["ones"], T["ident"]
    EXP = AF.Exp
    kval = sb("kval", [128, 2, 128], BF16)
    S.dma("sp", "c3", kval[:], T["kval"].ap(), writes=["kval"])
    kcT_all = sb("kcT_all", [128, 4, 256], BF16)
    vc_all = sb("vc_all", [128, 4, 2, 128], BF16)

    with ExitStack() as pb:
        XT = sb("XTB", [128, CTX + 32], BF16, pb)
        W1 = sb("W1B", [128, 32, 128], BF16, pb)
        W2 = sb("W2B", [128, 128], BF16, pb)
        pe32 = sb("pe32", [128, 32], F32, pb)
        pe16 = sb("pe16", [128, 32], BF16, pb)
        b1 = sb("b1B", [128, 1], F32, pb)
        xg = sb("xgB", [128, 256], F32, pb)
        x2 = sb("x2B", [128, 256], F32, pb)
        gTb = sb("gTB", [128, 256], BF16, pb)
        pb1 = ps("pb1", [128, 512], F32, pb)
        po1 = ps("po1", [128, 512], F32, pb)
        po2 = ps("po2", [128, 512], F32, pb)
        S.op("dve", lambda: nc.vector.memset(XT[:, CTX:CTX + 32], 0.0), writes=["XT"])
        for kind in range(2):
            S.dma("pool", "W1B", W1[:], T["cw1"][kind].ap().rearrange("(i d) h -> d i h", d=128), writes=["W1"])
            S.dma("pool", "W2B", W2[:], T["cw2"][kind].ap(), writes=["W2"])
            S.dma("sp", "pe32", pe32[:], T["cpeT"][kind].ap(), writes=["pe32"])
            S.op("dve", lambda: nc.vector.tensor_copy(out=pe16[:], in_=pe32[:]), reads=["pe32"], writes=["pe16"])
            for i in range(32):
                S.op("pe", lambda i=i: nc.tensor.matmul(pb1[:, 0:1], lhsT=W1[:, i, :], rhs=pe16[:, i:i + 1], start=(i == 0), stop=(i == 31)),
                     reads=["W1", "pe16"], writes=["pb1"])
            S.op("dve", lambda: nc.vector.tensor_copy(out=b1[:], in_=pb1[:, 0:1]), reads=["pb1"], writes=["b1"])
            for g in range(4):
                slot = (KT_KBC if kind == 0 else KT_VBC) + g
                S.dma("sp", "XTB", XT[:, 0:CTX], kT.ap()[slot, :, :], reads=["dram_qk"], writes=["XT"])
                for i in range(32):
                    rhs = dap(XT, i, [[CTX + 32, 128], [16, 256]])
                    S.op("pe", lambda i=i, rhs=rhs: nc.tensor.matmul(po1[:, 0:256], lhsT=W1[:, i, :], rhs=rhs, start=(i == 0), stop=(i == 31)),
                         reads=["W1", "XT"], writes=["po1"])
                S.op("dve", lambda: nc.vector.tensor_scalar(out=xg[:], in0=po1[:, 0:256], scalar1=b1[:, 0:1], scalar2=None, op0=ALU.add),
                     reads=["po1", "b1"], writes=["xg"])
                S.op("act", lambda: nc.scalar.activation(out=x2[:], in_=xg[:], func=AF.Square), reads=["xg"], writes=["x2"])
                S.op("dve", lambda: nc.vector.tensor_scalar(out=x2[:], in0=x2[:], scalar1=0.044715, scalar2=1.0, op0=ALU.mult, op1=ALU.add),
                     reads=["x2"], writes=["x2"])
                S.op("dve", lambda: nc.vector.tensor_tensor(out=x2[:], in0=x2[:], in1=xg[:], op=ALU.mult), reads=["x2", "xg"], writes=["x2"])
                S.op("act", lambda: nc.scalar.activation(out=x2[:], in_=x2[:], func=AF.Sigmoid, scale=1.5957691216057308),
                     reads=["x2"], writes=["x2"])
                S.op("dve", lambda: nc.vector.tensor_tensor(out=gTb[:], in0=x2[:], in1=xg[:], op=ALU.mult), reads=["x2", "xg"], writes=["gTb"])
                if kind == 0:
                    S.op("pe", lambda: nc.tensor.matmul(po2[:, 0:256], lhsT=W2[:], rhs=gTb[:], start=True, stop=True),
                         reads=["W2", "gTb"], writes=["po2"])
                    S.op("act", lambda g=g: nc.scalar.copy(out=kcT_all[:, g, :], in_=po2[:, 0:256]), reads=["po2"], writes=["kcT_all"])
                else:
                    for cb in range(2):
                        S.op("pe", lambda cb=cb: nc.tensor.matmul(po2[:, cb * 128:(cb + 1) * 128], lhsT=gTb[:, cb * 128:(cb + 1) * 128], rhs=W2[:],
                                                             start=True, stop=True), reads=["W2", "gTb"], writes=["po2"])
                    S.op("act", lambda g=g: nc.scalar.copy(out=vc_all[:, g, :, :], in_=po2[:, 0:256].rearrange("p (a b) -> p a b", a=2)),
                         reads=["po2"], writes=["vc_all"])
        S.barrier()

    def pair(ST, stn, lhsK, rhsQ, E, en, PT, pn, mask_ap, mreads, O, on, lhsV, vreads, Dn, dn, lhsD, dreads, first, last):
        S.op("pe", lambda: nc.tensor.matmul(ST[:], lhsT=lhsK, rhs=rhsQ, start=True, stop=True), reads=vreads[:1] + ["QT"], writes=[stn])
        S.op("act", lambda: nc.scalar.activation(out=E[:], in_=ST[:], func=EXP, scale=SCALE), reads=[stn], writes=[en])
        S.op("dve", lambda: nc.vector.tensor_tensor(out=PT[:], in0=E[:], in1=mask_ap, op=ALU.mult), reads=[en] + mreads, writes=[pn])
        S.op("pe", lambda: nc.tensor.matmul(O[:], lhsT=lhsV, rhs=PT[:], start=first, stop=last), reads=[pn] + vreads[1:], writes=[on])
        S.op("pe", lambda: nc.tensor.matmul(Dn[:], lhsT=lhsD, rhs=PT[:], start=first, stop=last), reads=[pn] + dreads, writes=[dn])

    with ExitStack() as pc:
        mA = sb("mA", [128, 3072], BF16, pc)
        S.dma("sp", "c4", mA[:], T["mA"].ap(), writes=["mA"])
        KT = [sb("KTA%d" % i, [128, CTX], BF16, pc) for i in range(2)]
        VH = [sb("VHA%d" % i, [128, 32, 128], BF16, pc) for i in range(2)]
        QT = [sb("QTA%d" % i, [128, OWN], BF16, pc) for i in range(2)]
        E = [sb("EA%d" % i, [128, TT], BF16, pc) for i in range(3)]
        PT = [sb("PTA%d" % i, [128, TT], BF16, pc) for i in range(3)]
        rden = [sb("rdA%d" % i, [128, TT], F32, pc) for i in range(2)]
        ob = [sb("obA%d" % i, [128, TT], BF16, pc) for i in range(2)]
        ST = [ps("STA%d" % i, [128, TT], F32, pc) for i in range(3)]
        O = [ps("OA%d" % i, [128, TT], F32, pc) for i in range(2)]
        Dn = [ps("DA%d" % i, [128, TT], F32, pc) for i in range(2)]
        pi = 0
        oi = 0
        for h in range(16):
            u = h % 2
            S.dma("sp", "KTA%d" % u, KT[u][:], kT.ap()[KT_KA + h, :, :], reads=["dram_qk"], writes=["KT%d" % u])
            S.dma("sp", "VHA%d" % u, VH[u][:], vv.ap()[:, V_VA + h * 128:V_VA + (h + 1) * 128].rearrange("(kb p) d -> p kb d", p=128),
                  reads=["dram_v"], writes=["VH%d" % u])
            S.dma("sp", "QTA%d" % u, QT[u][:], qT.ap()[h, :, :], reads=["dram_qk"], writes=["QT%d" % u])
            for t in tiles_c:
                o = oi % 2
                oi += 1
                kbs = list(range(4 * t, 4 * t + 20))
                for n, kb in enumerate(kbs):
                    D = 16 + 4 * t - kb
                    s3 = pi % 3
                    pi += 1
                    S.op("pe", lambda kb=kb, s3=s3: nc.tensor.matmul(ST[s3][:], lhsT=KT[u][:, kb * 128:(kb + 1) * 128],
                                                                 rhs=QT[u][:, t * TT:(t + 1) * TT], start=True, stop=True),
                         reads=["KT%d" % u, "QT%d" % u], writes=["STA%d" % s3])
                    S.op("act", lambda s3=s3: nc.scalar.activation(out=E[s3][:], in_=ST[s3][:], func=EXP, scale=SCALE),
                         reads=["STA%d" % s3], writes=["EA%d" % s3])
                    S.op("dve", lambda s3=s3, D=D: nc.vector.tensor_tensor(out=PT[s3][:], in0=E[s3][:], in1=mA[:, 128 * D + 384:128 * D + 896], op=ALU.mult),
                         reads=["EA%d" % s3, "mA"], writes=["PTA%d" % s3])
                    S.op("pe", lambda kb=kb, s3=s3, n=n: nc.tensor.matmul(O[o][:], lhsT=VH[u][:, kb, :], rhs=PT[s3][:], start=(n == 0), stop=(n == 19)),
                         reads=["PTA%d" % s3, "VH%d" % u], writes=["OA%d" % o])
                    S.op("pe", lambda kb=kb, s3=s3, n=n: nc.tensor.matmul(Dn[o][:], lhsT=kval[:, 0 if kb < 16 else 1, :], rhs=PT[s3][:],
                                                                      start=(n == 0), stop=(n == 19)),
                         reads=["PTA%d" % s3, "kval"], writes=["DA%d" % o])
                S.op("dve", lambda o=o: nc.vector.reciprocal(out=rden[o][:], in_=Dn[o][:]), reads=["DA%d" % o], writes=["rdA%d" % o])
                S.op("dve", lambda o=o: nc.vector.tensor_tensor(out=ob[o][:], in0=O[o][:], in1=rden[o][:], op=ALU.mult),
                     reads=["OA%d" % o, "rdA%d" % o], writes=["obA%d" % o])
                S.dma("sp", "obA%d" % o, oT.ap()[h, :, t * TT:(t + 1) * TT], ob[o][:], reads=["obA%d" % o], writes=["dram_oT"])
        S.barrier()

    with ExitStack() as pn_:
        st = pn_
        mW = sb("mW", [128, 1408], BF16, st)
        mC = sb("mC", [128, 1024], BF16, st)
        cmask = sb("cmask", [128, 2, OWN], BF16, st)
        span = sb("span", [128, 2, 64], BF16, st)
        VM = sb("VM", [128, 16, 64], F32, st)
        FB = sb("FB", [128, 16, 64], F32, st)
        eexp = sb("eexp", [128, 32, 128], BF16, st)
        for nm, tl in (("mW", mW), ("mC", mC), ("cmask", cmask), ("span", span), ("VM", VM), ("FB", FB), ("eexp", eexp)):
            S.dma("sp", "k_" + nm, tl[:], T[nm].ap(), writes=[nm])
        QB = sb("QB", [128, 4, OWN], BF16, st)
        KS = sb("KS", [128, CTX], BF16, st)
        VS = sb("VS", [128, 32, 128], BF16, st)
        KW = sb("KW", [128, CTX], BF16, st)
        VW = sb("VW", [128, 32, 128], BF16, st)
        EC = sb("EC", [128, 4, 2, TT], BF16, st)
        SM = sb("SM", [128, 32, TT], BF16, st)
        accO = [sb("accO%d" % i, [128, TT], F32, st) for i in range(4)]
        G = [sb("G%d" % i, [128, 3, TT], F32, st) for i in range(4)]
        E = [sb("EB%d" % i, [128, TT], BF16, st) for i in range(3)]
        PT = [sb("PTB%d" % i, [128, TT], BF16, st) for i in range(3)]
        rcb = sb("rcb", [128, TT], F32, st)
        coef = sb("coef", [128, TT], F32, st)
        tmpo = sb("tmpo", [128, TT], F32, st)
        sc = sb("sc", [128, 4, 64], F32, st)
        sc2 = sb("sc2", [128, 64], F32, st)
        m8 = sb("m8", [128, 8], F32, st)
        selp = sb("selp", [128, 4, 128], BF16, st)
        self_ = sb("self", [128, 64], F32, st)
        selT = sb("selT", [128, TT], BF16, st)
        obb = [sb("obB%d" % i, [128, TT], BF16, st) for i in range(2)]
        ST = [ps("STB%d" % i, [128, TT], F32, st) for i in range(2)]
        O = ps("OB", [128, TT], F32, st)
        Dn = ps("DB", [128, TT], F32, st)
        IMP = ps("IMP", [128, TT], F32, st)
        TPS = ps("TPS", [128, 1024], BF16, st)
        MB = [ps("MB%d" % i, [128, TT], F32, st) for i in range(2)]
        S.op("dve", lambda: nc.vector.memset(selp[:], 0.0), writes=["selp"])
        pi = 0
        obi = 0

        def branch(r, t, Ksb, kname, Vsb, vname, kbs, mask_of, mreads, den_of, dreads):
            nonlocal pi
            for n, kb in enumerate(kbs):
                s2 = pi % 2
                s3 = pi % 3
                pi += 1
                S.op("pe", lambda: nc.tensor.matmul(ST[s2][:], lhsT=Ksb[:, kb * 128:(kb + 1) * 128], rhs=QB[:, r, t * TT:(t + 1) * TT],
                                                   start=True, stop=True), reads=[kname, "QB"], writes=["STB%d" % s2])
                S.op("act", lambda: nc.scalar.activation(out=E[s3][:], in_=ST[s2][:], func=EXP, scale=SCALE),
                     reads=["STB%d" % s2], writes=["EB%d" % s3])
                S.op("dve", lambda: nc.vector.tensor_tensor(out=PT[s3][:], in0=E[s3][:], in1=mask_of(kb), op=ALU.mult),
                     reads=["EB%d" % s3] + mreads, writes=["PTB%d" % s3])
                S.op("pe", lambda: nc.tensor.matmul(O[:], lhsT=Vsb[:, kb, :], rhs=PT[s3][:], start=(n == 0), stop=(n == len(kbs) - 1)),
                     reads=["PTB%d" % s3, vname], writes=["OB"])
                S.op("pe", lambda: nc.tensor.matmul(Dn[:], lhsT=den_of(kb), rhs=PT[s3][:], start=(n == 0), stop=(n == len(kbs) - 1)),
                     reads=["PTB%d" % s3] + dreads, writes=["DB"])

        def combine(r, br, first):
            S.op("dve", lambda: nc.vector.reciprocal(out=rcb[:], in_=Dn[:]), reads=["DB"], writes=["rcb"])
            S.op("dve", lambda: nc.vector.tensor_tensor(out=coef[:], in0=rcb[:], in1=G[r][:, br, :], op=ALU.mult),
                 reads=["rcb", "G%d" % r], writes=["coef"])
            if first:
                S.op("dve", lambda: nc.vector.tensor_tensor(out=accO[r][:], in0=O[:], in1=coef[:], op=ALU.mult),
                     reads=["OB", "coef"], writes=["accO%d" % r])
            else:
                S.op("dve", lambda: nc.vector.tensor_tensor(out=tmpo[:], in0=O[:], in1=coef[:], op=ALU.mult),
                     reads=["OB", "coef"], writes=["tmpo"])
                S.op("dve", lambda: nc.vector.tensor_tensor(out=accO[r][:], in0=accO[r][:], in1=tmpo[:], op=ALU.add),
                     reads=["tmpo", "accO%d" % r], writes=["accO%d" % r])

        for g in range(4):
            for r in range(4):
                S.dma("sp", "QB", QB[:, r, :], qT.ap()[16 + 4 * g + r, :, :], reads=["dram_qk"], writes=["QB"])
            S.dma("sp", "KS", KS[:], kT.ap()[KT_KBS + g, :, :], reads=["dram_qk"], writes=["KS"])
            S.dma("sp", "KW", KW[:], kT.ap()[KT_KBW + g, :, :], reads=["dram_qk"], writes=["KW"])
            S.dma("sp", "VS", VS[:], vv.ap()[:, V_VBS + g * 128:V_VBS + (g + 1) * 128].rearrange("(kb p) d -> p kb d", p=128),
                  reads=["dram_v"], writes=["VS"])
            S.dma("sp", "VW", VW[:], vv.ap()[:, V_VBW + g * 128:V_VBW + (g + 1) * 128].rearrange("(kb p) d -> p kb d", p=128),
                  reads=["dram_v"], writes=["VW"])
            for t in tiles_c:
                for r in range(4):
                    S.dma("sp", "G%d" % r, G[r][:], dap(gTd, ((4 * g + r) * 3) * OWN + t * TT, [[0, 128], [OWN, 3], [1, TT]]),
                          reads=["dram_g"], writes=["G%d" % r])
                    for cb in range(2):
                        s2 = pi % 2
                        s3 = pi % 3
                        pi += 1
                        S.op("pe", lambda cb=cb, s2=s2: nc.tensor.matmul(ST[s2][:], lhsT=kcT_all[:, g, cb * 128:(cb + 1) * 128],
                                                                     rhs=QB[:, r, t * TT:(t + 1) * TT], start=True, stop=True),
                             reads=["kcT_all", "QB"], writes=["STB%d" % s2])
                        S.op("act", lambda s2=s2, s3=s3: nc.scalar.activation(out=E[s3][:], in_=ST[s2][:], func=EXP, scale=SCALE),
                             reads=["STB%d" % s2], writes=["EB%d" % s3])
                        S.op("dve", lambda cb=cb, s3=s3: nc.vector.tensor_tensor(out=EC[:, r, cb, :], in0=E[s3][:], in1=cmask[:, cb, t * TT:(t + 1) * TT],
                                                                            op=ALU.mult), reads=["EB%d" % s3, "cmask"], writes=["EC"])
                        S.op("pe", lambda cb=cb: nc.tensor.matmul(O[:], lhsT=vc_all[:, g, cb, :], rhs=EC[:, r, cb, :], start=(cb == 0), stop=(cb == 1)),
                             reads=["EC", "vc_all"], writes=["OB"])
                        S.op("pe", lambda cb=cb: nc.tensor.matmul(Dn[:], lhsT=ones[:], rhs=EC[:, r, cb, :], start=(cb == 0), stop=(cb == 1)),
                             reads=["EC", "ones"], writes=["DB"])
                    S.op("dve", lambda: nc.vector.tensor_scalar(out=rcb[:], in0=Dn[:], scalar1=1e-30, scalar2=None, op0=ALU.max),
                         reads=["DB"], writes=["rcb"])
                    S.op("dve", lambda: nc.vector.reciprocal(out=rcb[:], in_=rcb[:]), reads=["rcb"], writes=["rcb"])
                    S.op("dve", lambda: nc.vector.tensor_tensor(out=coef[:], in0=rcb[:], in1=G[r][:, 0, :], op=ALU.mult),
                         reads=["rcb", "G%d" % r], writes=["coef"])
                    S.op("dve", lambda: nc.vector.tensor_tensor(out=accO[r][:], in0=O[:], in1=coef[:], op=ALU.mult),
                         reads=["OB", "coef"], writes=["accO%d" % r])
                    for cb in range(2):
                        S.op("dve", lambda cb=cb: nc.vector.tensor_tensor(out=EC[:, r, cb, :], in0=EC[:, r, cb, :], in1=rcb[:], op=ALU.mult),
                             reads=["EC", "rcb"], writes=["EC"])
                for qb in range(4):
                    n = 0
                    for r in range(4):
                        for cb in range(2):
                            S.op("pe", lambda qb=qb, r=r, cb=cb, n=n: nc.tensor.matmul(IMP[:, qb * 64:(qb + 1) * 64], lhsT=EC[:, r, cb, qb * 128:(qb + 1) * 128],
                                                                               rhs=span[:, cb, :], start=(n == 0), stop=(n == 7)),
                                 reads=["EC", "span"], writes=["IMP"])
                            n += 1
                S.op("dve", lambda: nc.vector.tensor_tensor(out=sc[:], in0=IMP[:, 0:256].rearrange("p (a b) -> p a b", a=4),
                                                            in1=VM[:, 4 * t:4 * t + 4, :], op=ALU.mult), reads=["IMP", "VM"], writes=["sc"])
                S.op("dve", lambda: nc.vector.tensor_tensor(out=sc[:], in0=sc[:], in1=FB[:, 4 * t:4 * t + 4, :], op=ALU.add),
                     reads=["sc", "FB"], writes=["sc"])
                for qb in range(4):
                    S.op("dve", lambda qb=qb: nc.vector.max(out=m8[:], in_=sc[:, qb, :]), reads=["sc"], writes=["m8"])
                    S.op("dve", lambda qb=qb: nc.vector.match_replace(out=sc2[:], in_to_replace=m8[:], in_values=sc[:, qb, :], imm_value=-3.0e38),
                         reads=["sc", "m8"], writes=["sc2"])
                    S.op("dve", lambda: nc.vector.max(out=m8[:], in_=sc2[:]), reads=["sc2"], writes=["m8"])
                    S.op("dve", lambda qb=qb: nc.vector.tensor_scalar(out=self_[:], in0=sc[:, qb, :], scalar1=m8[:, 7:8], scalar2=None, op0=ALU.is_ge),
                         reads=["sc", "m8"], writes=["self"])
                    S.op("dve", lambda qb=qb: nc.vector.tensor_tensor(out=selp[:, qb, 0:64], in0=self_[:], in1=VM[:, 4 * t + qb, :], op=ALU.mult),
                         reads=["self", "VM"], writes=["selp"])
                for qb in range(4):
                    S.op("pe", lambda qb=qb: nc.tensor.transpose(out=TPS[:, qb * 128:(qb + 1) * 128], in_=selp[:, qb, :], identity=ident[:]),
                         reads=["selp", "ident"], writes=["TPS"])
                S.op("act", lambda: nc.scalar.copy(out=selT[:], in_=TPS[:, 0:TT]), reads=["TPS"], writes=["selT"])
                nkb = 20 + 4 * t
                for kb in range(nkb):
                    D = min(16 + 4 * t - kb, 1)
                    mb = kb % 2
                    S.op("pe", lambda kb=kb, mb=mb: nc.tensor.matmul(MB[mb][:], lhsT=eexp[:, kb, :], rhs=selT[:], start=True, stop=True),
                         reads=["eexp", "selT"], writes=["MB%d" % mb])
                    S.op("dve", lambda kb=kb, mb=mb, D=D: nc.vector.tensor_tensor(out=SM[:, kb, :], in0=MB[mb][:], in1=mC[:, 128 * D + 384:128 * D + 896],
                                                                             op=ALU.mult), reads=["MB%d" % mb, "mC"], writes=["SM"])
                for r in range(4):
                    branch(r, t, KS, "KS", VS, "VS", list(range(nkb)), lambda kb: SM[:, kb, :], ["SM"], lambda kb: ones[:], ["ones"])
                    combine(r, 1, False)
                    branch(r, t, KW, "KW", VW, "VW", list(range(4 * t + 12, 4 * t + 20)),
                           lambda kb: mW[:, 128 * (16 + 4 * t - kb) + 384:128 * (16 + 4 * t - kb) + 896], ["mW"],
                           lambda kb: kval[:, 0 if kb < 16 else 1, :], ["kval"])
                    combine(r, 2, False)
                    o = obi % 2
                    obi += 1
                    S.op("act", lambda o=o, r=r: nc.scalar.copy(out=obb[o][:], in_=accO[r][:]), reads=["accO%d" % r], writes=["obB%d" % o])
                    S.dma("sp", "obB%d" % o, oT.ap()[16 + 4 * g + r, :, t * TT:(t + 1) * TT], obb[o][:], reads=["obB%d" % o], writes=["dram_oT"])
        S.barrier()


def ffn_tile(nc, S, stack, sb, ps, t, q0, h2T, w_gate, w_up, w_down, x1d, g_fin, y, epsb):
    with ExitStack() as p4:
        actT = sb("actT", [128, 86, TT], BF16, p4)
        with ExitStack() as p4a:
            Wg = [sb("WgD%d" % i, [128, 32, 256], BF16, p4a) for i in range(2)]
            Wu = [sb("WuD%d" % i, [128, 32, 256], BF16, p4a) for i in range(2)]
            sl = [sb("slD%d" % i, [128, TT], F32, p4a) for i in range(2)]
            gb = [ps("gbD%d" % i, [128, TT], F32, p4a) for i in range(2)]
            ub = [ps("ubD%d" % i, [128, TT], F32, p4a) for i in range(2)]
            for fb in range(43):
                wg, wu = Wg[fb % 2], Wu[fb % 2]
                gn, un_ = "WgD%d" % (fb % 2), "WuD%d" % (fb % 2)
                f0 = fb * 256
                gsrc = w_gate.ap()[:, f0:f0 + 256].rearrange("(kc p) c -> p kc c", p=128)
                usrc = w_up.ap()[:, f0:f0 + 256].rearrange("(kc p) c -> p kc c", p=128)
                for k4 in range(4):
                    S.dma("pool", gn, wg[:, 8 * k4:8 * k4 + 8, :], gsrc[:, 8 * k4:8 * k4 + 8, :], writes=[gn])
                    S.dma("pool", un_, wu[:, 8 * k4:8 * k4 + 8, :], usrc[:, 8 * k4:8 * k4 + 8, :], writes=[un_])
                for j in range(2):
                    fc = fb * 2 + j
                    b = fc % 2
                    for kc in range(32):
                        S.op("pe", lambda kc=kc: nc.tensor.matmul(gb[b][:], lhsT=wg[:, kc, j * 128:(j + 1) * 128], rhs=h2T[:, kc, :],
                                                             start=(kc == 0), stop=(kc == 31)), reads=[gn, "hT"], writes=["gb%d" % b])
                    for kc in range(32):
                        S.op("pe", lambda kc=kc: nc.tensor.matmul(ub[b][:], lhsT=wu[:, kc, j * 128:(j + 1) * 128], rhs=h2T[:, kc, :],
                                                             start=(kc == 0), stop=(kc == 31)), reads=[un_, "hT"], writes=["ub%d" % b])
                    S.op("act", lambda: nc.scalar.activation(out=sl[b][:], in_=gb[b][:], func=AF.Silu), reads=["gb%d" % b], writes=["sl%d" % b])
                    S.op("dve", lambda: nc.vector.tensor_tensor(out=actT[:, fc, :], in0=ub[b][:], in1=sl[b][:], op=ALU.mult),
                         reads=["ub%d" % b, "sl%d" % b], writes=["actT"])
            S.barrier()
        with ExitStack() as p5:
            Wd = [sb("WdD%d" % i, [128, 4, 512], BF16, p5) for i in range(2)]
            x2 = sb("x2D", [128, 2, 4096], F32, p5)
            x1r = sb("x1rD", [128, 2, 512], F32, p5)
            gfin = sb("gfinD", [128, 4096], F32, p5)
            junk = sb("junkD", [128, 4096], BF16, p5)
            ssq = sb("ssq5", [128, 1], F32, p5)
            rstd = sb("rstd5", [128, 1], F32, p5)
            acc = [ps("acc5_%d" % i, [128, 512], F32, p5) for i in range(4)]
            S.dma("sp", "gain", gfin[:], dap(g_fin, 0, [[0, 128], [1, 4096]]), writes=["gain"])
            wi = 0
            for hf in range(2):
                r_lo = q0 + hf * 256
                for cb in range(8):
                    c0 = cb * 512
                    S.dma("sp", "x1r", x1r[:], x1d.ap()[r_lo:r_lo + 256, c0:c0 + 512].rearrange("(b p) c -> p b c", p=128),
                          reads=["dram_x1"], writes=["x1r"])
                    for f4 in range(22):
                        nf = 4 if f4 < 21 else 2
                        w = Wd[wi % 2]
                        wn = "WdD%d" % (wi % 2)
                        wi += 1
                        r0 = f4 * 4 * 128
                        wsrc = w_down.ap()[r0:r0 + nf * 128, c0:c0 + 512].rearrange("(f p) c -> p f c", p=128)
                        S.dma("pool", wn, w[:, 0:nf, :], wsrc, writes=[wn])
                        for fl in range(nf):
                            fc = f4 * 4 + fl
                            for tb in range(2):
                                bk = (cb % 2) * 2 + tb
                                tg = hf * 2 + tb
                                S.op("pe", lambda fl=fl, fc=fc, tg=tg, bk=bk: nc.tensor.matmul(
                                    acc[bk][:], lhsT=actT[:, fc, tg * 128:(tg + 1) * 128], rhs=w[:, fl, :],
                                    start=(fc == 0), stop=(fc == 85)), reads=["actT", wn], writes=["acc5_%d" % bk])
                    for tb in range(2):
                        bk = (cb % 2) * 2 + tb
                        S.op("dve", lambda tb=tb, bk=bk: nc.vector.tensor_tensor(out=x2[:, tb, c0:c0 + 512], in0=acc[bk][:], in1=x1r[:, tb, :], op=ALU.add),
                             reads=["acc5_%d" % bk, "x1r"], writes=["x2"])
                for tb in range(2):
                    S.op("act", lambda tb=tb: nc.scalar.activation(out=junk[:], in_=x2[:, tb, :], func=AF.Square, scale=1.0 / 64.0, accum_out=ssq[:]),
                         reads=["x2"], writes=["junk", "ssq"])
                    S.op("act", lambda: nc.scalar.activation(out=ssq[:], in_=ssq[:], func=AF.Sqrt, bias=epsb[:]), reads=["ssq", "epsb"], writes=["ssq"])
                    S.op("dve", lambda: nc.vector.reciprocal(out=rstd[:], in_=ssq[:]), reads=["ssq"], writes=["rstd"])
                    S.op("dve", lambda tb=tb: nc.vector.scalar_tensor_tensor(out=x2[:, tb, :], in0=x2[:, tb, :], scalar=rstd[:, 0:1], in1=gfin[:],
                                                                        op0=ALU.mult, op1=ALU.mult), reads=["x2", "rstd", "gain"], writes=["x2"])
                S.dma("sp", "yo", y.ap()[r_lo:r_lo + 256, :].rearrange("(b p) c -> p b c", p=128), x2[:], reads=["x2"], writes=["dram_y"])
            S.barrier()


def finish(nc, S, stack, y):
    S.final_wait("sp")
    stack.close()
    return nc


def _consts(half):
    p = np.arange(128)[:, None]
    c = {}
    u = np.arange(3072)[None, :]
    d = u - 384 - p
    m = ((d >= 0) & (d <= 128)).astype(np.float32) + ((d >= 0) & (d <= 512) & (d % 4 == 0)) + \
        ((d >= 0) & (d <= 2048) & (d % 16 == 0))
    c["mA"] = m.astype(NPBF)
    u = np.arange(1408)[None, :]
    d = u - 384 - p
    c["mW"] = ((d >= 0) & (d <= 511)).astype(NPBF)
    u = np.arange(1024)[None, :]
    d = u - 384 - p
    c["mC"] = (d >= 0).astype(NPBF)
    cp = np.arange(256)
    qctx = OWN + np.arange(OWN)
    cvalid = (cp <= 254) & ((cp >= 128) | (half == 1))
    cm = ((16 * cp[:, None] + 31) <= qctx[None, :]) & cvalid[:, None]
    c["cmaskT"] = np.ascontiguousarray(cm.reshape(2, 128, OWN).transpose(1, 0, 2)).astype(NPBF)
    j = np.arange(64)
    sh = ((16 * cp[:, None]) < (64 * j[None, :] + 64)) & ((16 * cp[:, None] + 32) > 64 * j[None, :])
    c["span"] = np.ascontiguousarray(sh.reshape(2, 128, 64).transpose(1, 0, 2)).astype(NPBF)
    jabs = j[None, :] - (32 if half == 0 else 0)
    qblk_ctx = (qctx // 64)[:, None]
    j0 = 32 if half == 0 else 0
    valid = (j[None, :] * 64 <= qctx[:, None]) & (jabs >= 0)
    forced = ((j[None, :] == j0) | (j[None, :] == qblk_ctx) | (j[None, :] == qblk_ctx - 1)) & valid
    VM = valid.astype(np.float32)
    FB = np.where(forced, 1e6, np.where(valid, 0.0, -1e30)).astype(np.float32)
    c["VM"] = np.ascontiguousarray(VM.reshape(16, 128, 64).transpose(1, 0, 2))
    c["FB"] = np.ascontiguousarray(FB.reshape(16, 128, 64).transpose(1, 0, 2))
    kb = np.arange(32)
    k = np.arange(128)
    ee = (j[:, None, None] == (2 * kb[None, :, None] + (k[None, None, :] // 64)))
    c["eexp"] = np.concatenate([ee, np.zeros_like(ee)], 0).astype(NPBF)
    kv = np.ones((128, 2, 128), np.float32)
    kv[:, 0, :] = 1.0 if half == 1 else 0.0
    c["kval"] = kv.astype(NPBF)
    c["ident"] = np.eye(128, dtype=np.float32).astype(NPBF)
    r = np.zeros((128, 128), np.float32)
    for mm in range(32):
        r[(mm + 16) % 32, mm] = 1.0
    c["rm"] = r.astype(NPBF)
    pos = (np.arange(CTX) - OWN + half * OWN).astype(np.float32)
    inv = (np.float32(500000.0) ** (-np.arange(0, 32, 2, dtype=np.float32) / np.float32(32))).astype(np.float32)
    ang = pos[None, :] * inv[:, None]
    cs, sn = np.cos(ang).astype(np.float32), np.sin(ang).astype(np.float32)
    c["ropeC"] = np.concatenate([cs, cs, np.ones((96, CTX), np.float32)], 0)
    c["ropeS"] = np.concatenate([-sn, sn, np.zeros((96, CTX), np.float32)], 0)
    return c


def prep_core(inp, b, half):
    f = lambda a: np.ascontiguousarray(np.asarray(a, dtype=np.float32))
    x = np.asarray(inp["x"])
    xcv = np.zeros((CTX, DM), np.float32)
    if half == 1:
        xcv[:] = x[b]
    else:
        xcv[OWN:] = x[b, :OWN]
    m = {"xc": xcv, "w_in": f(inp["w_in"][0]), "w_out": f(inp["w_out"][0]), "w_gate": f(inp["w_gate"][0]),
         "w_up": f(inp["w_up"][0]), "w_down": f(inp["w_down"][0]),
         "ck_w1": f(inp["ck_w1"][0]), "cv_w1": f(inp["cv_w1"][0]), "ck_w2": f(inp["ck_w2"][0]),
         "cv_w2": f(inp["cv_w2"][0]), "ck_peT": f(np.asarray(inp["ck_pe"][0]).T), "cv_peT": f(np.asarray(inp["cv_pe"][0]).T),
         "g_attn": f(inp["norm_attn"][0]).reshape(1, DM), "g_ffn": f(inp["norm_ffn"][0]).reshape(1, DM),
         "g_fin": f(inp["norm_final"]).reshape(1, DM),
         "g_outT": f(np.concatenate([np.asarray(inp["out_norm_a"][0]), np.asarray(inp["out_norm_b"][0])]).reshape(32, 128).T)}
    m.update(_consts(half))
    return m


def kernel(**inputs):
    nc = build_program()
    in_maps = [prep_core(inputs, c // 2, c % 2) for c in range(8)]
    res = run_bass_kernel_spmd(nc, in_maps, core_ids=list(range(8)))
    out = np.zeros((4, SEQ, DM), np.float32)
    for c in range(8):
        out[c // 2, (c % 2) * OWN:(c % 2 + 1) * OWN] = res.results[c]["y"]
    return out
```

```python
import os
import numpy as np
import ml_dtypes
from contextlib import ExitStack
import concourse.bass as bass
import concourse.mybir as mybir
from concourse.bass_utils import run_bass_kernel_spmd

F32 = mybir.dt.float32
BF16 = mybir.dt.bfloat16
AF = mybir.ActivationFunctionType
ALU = mybir.AluOpType
AX = mybir.AxisListType
NPBF = ml_dtypes.bfloat16

DM = 4096
SEQ = 4096
OWN = 2048
CTX = 4096
DFF = 11008
DIN = 11312
EPS = 1e-5
SCALE = 128 ** -0.5
NTT = 4
TT = 512


class Sched:
    def __init__(self, nc, stack):
        self.nc = nc
        self.stack = stack
        self.eng = {"pe": nc.tensor, "dve": nc.vector, "act": nc.scalar, "pool": nc.gpsimd, "sp": nc.sync}
        self.sem = {}
        self.cnt = {}
        self.known = {e: {} for e in self.eng}
        self.lastw = {}
        self.readers = {}
        self.nwaits = 0

    def _key(self, key):
        if key not in self.sem:
            self.sem[key] = self.stack.enter_context(self.nc.semaphore("s_" + key))
            self.cnt[key] = 0
        return key

    def _collect(self, e, reads, writes):
        deps = {}

        def need(k, v, raw):
            if k == e and not raw:
                return
            if deps.get(k, 0) < v:
                deps[k] = v

        for r in reads:
            w = self.lastw.get(r)
            if w:
                need(w[0], w[1], True)
        for r in writes:
            w = self.lastw.get(r)
            if w:
                need(w[0], w[1], False)
            for k, v in self.readers.get(r, {}).items():
                need(k, v, False)
        for k, v in deps.items():
            if self.known[e].get(k, 0) < v:
                self.eng[e].wait_ge(self.sem[k], v)
                self.known[e][k] = v
                self.nwaits += 1

    def _record(self, key, reads, writes):
        v = self.cnt[key]
        for r in reads:
            self.readers.setdefault(r, {})[key] = v
        for w in writes:
            self.lastw[w] = (key, v)
            self.readers[w] = {}

    def op(self, e, fn, reads=(), writes=()):
        self._key(e)
        self._collect(e, reads, writes)
        ins = fn()
        self.cnt[e] += 1
        ins.then_inc(self.sem[e], 1)
        self._record(e, reads, writes)

    def dma(self, q, key, out, in_, reads=(), writes=()):
        key = self._key("d_" + key)
        self._collect(q, reads, writes)
        ins = self.eng[q].dma_start(out=out, in_=in_)
        self.cnt[key] += 16
        ins.then_inc(self.sem[key], 16)
        self._record(key, reads, writes)

    def barrier(self, engines=("pe", "dve", "act", "pool", "sp")):
        for e in engines:
            for k, v in self.cnt.items():
                if k == e or k.startswith("d_cast"):
                    continue
                if self.known[e].get(k, 0) < v:
                    self.eng[e].wait_ge(self.sem[k], v)
                    self.known[e][k] = v

    def final_wait(self, e="sp"):
        for k, v in self.cnt.items():
            if k.startswith("d_") and self.known[e].get(k, 0) < v:
                self.eng[e].wait_ge(self.sem[k], v)
                self.known[e][k] = v


def dap(t, offset, pattern):
    return bass.AP(t, offset, [list(p) for p in pattern])


C_QA, C_KA, C_VA, C_QB = 0, 2048, 4096, 6144
C_KBC, C_VBC, C_KBS, C_VBS, C_KBW, C_VBW, C_GL = 8192, 8704, 9216, 9728, 10240, 10752, 11264
KT_KA, KT_KBC, KT_KBS, KT_KBW, KT_VBC = 0, 16, 20, 24, 28
V_VA, V_VBS, V_VBW = 0, 2048, 2560


def build_program(debug=False, stop_after=None, tiles=range(8), tiles_d=range(4), tiles_c=range(4), skip_attn=False):
    nc = bass.Bass("TRN2", target_bir_lowering=False)
    stack = ExitStack()
    S = Sched(nc, stack)
    dk = "ExternalOutput" if debug else "Internal"

    def din(name, shape, dt=F32):
        return nc.dram_tensor(name, list(shape), dt, kind="ExternalInput")

    xc = din("xc", [CTX, DM])
    w_in = din("w_in", [DM, DIN])
    w_out = din("w_out", [DM, DM])
    w_gate = din("w_gate", [DM, DFF])
    w_up = din("w_up", [DM, DFF])
    w_down = din("w_down", [DFF, DM])
    cw1 = [din("ck_w1", [4096, 128]), din("cv_w1", [4096, 128])]
    cw2 = [din("ck_w2", [128, 128]), din("cv_w2", [128, 128])]
    cpeT = [din("ck_peT", [128, 32]), din("cv_peT", [128, 32])]
    g_attn = din("g_attn", [1, DM])
    g_ffn = din("g_ffn", [1, DM])
    g_fin = din("g_fin", [1, DM])
    g_outT = din("g_outT", [128, 32])
    ropeC = din("ropeC", [128, CTX])
    ropeS = din("ropeS", [128, CTX])
    mA_d = din("mA", [128, 3072], BF16)
    mW_d = din("mW", [128, 1408], BF16)
    mC_d = din("mC", [128, 1024], BF16)
    cmask_d = din("cmaskT", [128, 2, OWN], BF16)
    span_d = din("span", [128, 2, 64], BF16)
    VM_d = din("VM", [128, 16, 64])
    FB_d = din("FB", [128, 16, 64])
    eexp_d = din("eexp", [128, 32, 128], BF16)
    kval_d = din("kval", [128, 2, 128], BF16)
    ident_d = din("ident", [128, 128], BF16)
    rm_d = din("rm", [128, 128], BF16)
    y = nc.dram_tensor("y", [OWN, DM], F32, kind="ExternalOutput")
    qT = nc.dram_tensor("qT", [32, 128, OWN], BF16, kind=dk)
    kT = nc.dram_tensor("kT", [32, 128, CTX], BF16, kind=dk)
    vv = nc.dram_tensor("vv", [CTX, 3072], BF16, kind=dk)
    gTd = nc.dram_tensor("gTd", [48, OWN], F32, kind=dk)
    kcT_d = nc.dram_tensor("kcT", [4, 128, 256], BF16, kind=dk)
    vc_d = nc.dram_tensor("vc", [4, 256, 128], BF16, kind=dk)
    oT = nc.dram_tensor("oT", [32, 128, OWN], BF16, kind=dk)
    x1d = nc.dram_tensor("x1d", [OWN, DM], F32, kind=dk)
    wb_in = nc.dram_tensor("wb_in", [23, 128, 32 * 512], BF16, kind="Internal")
    wb_o = nc.dram_tensor("wb_o", [16, 128, 32 * 256], BF16, kind="Internal")
    wb_g = nc.dram_tensor("wb_g", [43, 128, 32 * 256], BF16, kind="Internal")
    wb_u = nc.dram_tensor("wb_u", [43, 128, 32 * 256], BF16, kind="Internal")
    wb_d = nc.dram_tensor("wb_d", [8, 22, 128, 4 * 512], BF16, kind="Internal")
    ck = [0]

    def mk_cast(dst3, src3, res, nchunk, dimlen):
        def job():
            key = "cast%d" % (ck[0] % 3)
            ck[0] += 1
            dk_ = "d_" + key
            if dk_ in S.cnt and S.known["pool"].get(dk_, 0) < S.cnt[dk_]:
                S.eng["pool"].wait_ge(S.sem[dk_], S.cnt[dk_])
                S.known["pool"][dk_] = S.cnt[dk_]
            step = dimlen // nchunk
            for k in range(nchunk):
                S.dma("pool", key, dst3[:, k * step:(k + 1) * step, :], src3[:, k * step:(k + 1) * step, :], writes=[res])
        return job

    cast_q = []
    for cb in range(16):
        cast_q.append(mk_cast(wb_o.ap()[cb].rearrange("p (h c) -> p h c", h=32),
                              w_out.ap()[:, cb * 256:(cb + 1) * 256].rearrange("(h p) c -> p h c", p=128), "wbo%d" % cb, 4, 32))
    for fb in range(43):
        cast_q.append(mk_cast(wb_g.ap()[fb].rearrange("p (h c) -> p h c", h=32),
                              w_gate.ap()[:, fb * 256:(fb + 1) * 256].rearrange("(h p) c -> p h c", p=128), "wbg%d" % fb, 4, 32))
        cast_q.append(mk_cast(wb_u.ap()[fb].rearrange("p (h c) -> p h c", h=32),
                              w_up.ap()[:, fb * 256:(fb + 1) * 256].rearrange("(h p) c -> p h c", p=128), "wbu%d" % fb, 4, 32))
    cast_d = []
    for cb in range(8):
        for f4 in range(22):
            nf = 4 if f4 < 21 else 2
            r0 = f4 * 4 * 128
            cast_d.append(mk_cast(wb_d.ap()[cb, f4].rearrange("p (f c) -> p f c", f=4)[:, 0:nf, :],
                                  w_down.ap()[r0:r0 + nf * 128, cb * 512:(cb + 1) * 512].rearrange("(f p) c -> p f c", p=128),
                                  "wbd%d_%d" % (cb, f4), 1, nf))

    uid = [0]

    def sb(name, shape, dt, st=None):
        uid[0] += 1
        return (st or stack).enter_context(nc.sbuf_tensor("sb%d_%s" % (uid[0], name), list(shape), dt))

    def ps(name, shape, dt, st=None):
        uid[0] += 1
        return (st or stack).enter_context(nc.psum_tensor("ps%d_%s" % (uid[0], name), list(shape), dt))

    ident = sb("ident", [128, 128], BF16)
    rm = sb("rm", [128, 128], BF16)
    ones = sb("ones", [128, 128], BF16)
    S.dma("sp", "c0", ident[:], ident_d.ap(), writes=["ident"])
    S.dma("sp", "c1", rm[:], rm_d.ap(), writes=["rm"])
    S.op("dve", lambda: nc.vector.memset(ones[:], 1.0), writes=["ones"])
    epsb = sb("epsb", [128, 1], F32)
    S.op("dve", lambda: nc.vector.memset(epsb[:], EPS), writes=["epsb"])

    def norm_transpose(st_name, xin, gain_bc, xs, hT, blk, tp, ssq, rstd, tpi):
        S.op("act", lambda: nc.scalar.activation(out=xs[:], in_=xin, func=AF.Square, scale=1.0 / 64.0, accum_out=ssq[:]),
             reads=[st_name], writes=["xs", "ssq"])
        S.op("act", lambda: nc.scalar.activation(out=ssq[:], in_=ssq[:], func=AF.Sqrt, bias=epsb[:]),
             reads=["ssq", "epsb"], writes=["ssq"])
        S.op("dve", lambda: nc.vector.reciprocal(out=rstd[:], in_=ssq[:]), reads=["ssq"], writes=["rstd"])
        S.op("dve", lambda: nc.vector.scalar_tensor_tensor(out=xs[:], in0=xin, scalar=rstd[:, 0:1], in1=gain_bc[:],
                                                           op0=ALU.mult, op1=ALU.mult),
             reads=[st_name, "rstd", "gain"], writes=["xs"])
        for g8 in range(4):
            t = tp[tpi[0] % 2]
            tn = "tp%d" % (tpi[0] % 2)
            tpi[0] += 1
            for j in range(8):
                kc = g8 * 8 + j
                S.op("pe", lambda kc=kc, j=j, t=t: nc.tensor.transpose(out=t[:, j * 128:(j + 1) * 128],
                                                                     in_=xs[:, kc * 128:(kc + 1) * 128],
                                                                     identity=ident[:]),
                     reads=["xs", "ident"], writes=[tn])
            dst = hT[:, g8 * 8:(g8 + 1) * 8, blk * 128:(blk + 1) * 128]
            src = t[:].rearrange("p (a b) -> p a b", a=8)
            if g8 % 2 == 0:
                S.op("act", lambda dst=dst, src=src: nc.scalar.copy(out=dst, in_=src), reads=[tn], writes=["hT"])
            else:
                S.op("dve", lambda dst=dst, src=src: nc.vector.tensor_copy(out=dst, in_=src), reads=[tn], writes=["hT"])

    with ExitStack() as pa:
        gain = sb("gainA", [128, DM], F32, pa)
        xblk = [sb("xblk%d" % i, [128, DM], F32, pa) for i in range(2)]
        xs = sb("xsA", [128, DM], BF16, pa)
        hT = sb("hTA", [128, 32, TT], BF16, pa)
        Wt = [sb("WtA%d" % i, [128, 32, 512], BF16, pa) for i in range(2)]
        rC = sb("rC", [128, TT], F32, pa)
        rS = sb("rS", [128, TT], F32, pa)
        qsb = [sb("qsb%d" % i, [128, TT], BF16, pa) for i in range(4)]
        t1 = [sb("t1_%d" % i, [128, TT], F32, pa) for i in range(2)]
        t2 = [sb("t2_%d" % i, [128, TT], F32, pa) for i in range(2)]
        gsb = sb("gsb", [128, TT], F32, pa)
        ssq = sb("ssqA", [128, 1], F32, pa)
        rstd = sb("rstdA", [128, 1], F32, pa)
        tp = [ps("tpA%d" % i, [128, 1024], BF16, pa) for i in range(2)]
        acc = [ps("accA%d" % i, [128, 512], F32, pa) for i in range(4)]
        ps2 = ps("ps2A", [128, 512], F32, pa)

        S.dma("sp", "gain", gain[:], dap(g_attn, 0, [[0, 128], [1, DM]]), writes=["gain"])
        tpi = [0]
        wi = [0]
        acci = [0]
        qi = [0]
        ti = [0]
        blocks = []
        for j in range(4):
            blocks.append((C_QA + 512 * j, 512, "q", 4 * j, True, True))
        for j in range(4):
            blocks.append((C_KA + 512 * j, 512, "k", KT_KA + 4 * j, True, False))
        for j in range(4):
            blocks.append((C_VA + 512 * j, 512, "v", V_VA + 512 * j, False, False))
        for j in range(4):
            blocks.append((C_QB + 512 * j, 512, "q", 16 + 4 * j, True, True))
        blocks.append((C_KBC, 512, "k", KT_KBC, True, False))
        blocks.append((C_VBC, 512, "k", KT_VBC, False, False))
        blocks.append((C_KBS, 512, "k", KT_KBS, True, False))
        blocks.append((C_VBS, 512, "v", V_VBS, False, False))
        blocks.append((C_KBW, 512, "k", KT_KBW, True, False))
        blocks.append((C_VBW, 512, "v", V_VBW, False, False))
        blocks.append((C_GL, 48, "g", 0, False, True))

        cast_done = set()
        for tile in tiles:
            own = tile >= 4
            tok0 = tile * TT
            for blk in range(4):
                xb = xblk[(tile * 4 + blk) % 2]
                xn = "xblk%d" % ((tile * 4 + blk) % 2)
                r0 = tok0 + blk * 128
                S.dma("sp", xn, xb[:], xc.ap()[r0:r0 + 128, :], writes=[xn])
                norm_transpose(xn, xb[:], gain, xs, hT, blk, tp, ssq, rstd, tpi)
            S.dma("sp", "rC", rC[:], ropeC.ap()[:, tok0:tok0 + TT], writes=["rC"])
            S.dma("sp", "rS", rS[:], ropeS.ap()[:, tok0:tok0 + TT], writes=["rS"])

            def post_fm(bank, bn, kind, dest, rope, hh):
                if os.environ.get("KNOPOST"):
                    return
                if os.environ.get("KNOROPE"):
                    rope = False
                s = qi[0] % 4
                qi[0] += 1
                q = qsb[s]
                qn = "qsb%d" % s
                S.op("act", lambda: nc.scalar.copy(out=q[:], in_=bank[:]), reads=[bn], writes=[qn])
                if rope:
                    u = ti[0] % 2
                    ti[0] += 1
                    S.op("pe", lambda: nc.tensor.matmul(ps2[:], lhsT=rm[:], rhs=q[:], start=True, stop=True),
                         reads=[qn, "rm"], writes=["ps2"])
                    S.op("dve", lambda: nc.vector.tensor_tensor(out=t1[u][:], in0=bank[:], in1=rC[:], op=ALU.mult),
                         reads=[bn, qn, "rC"], writes=["t1_%d" % u])
                    S.op("dve", lambda: nc.vector.tensor_tensor(out=t2[u][:], in0=ps2[:], in1=rS[:], op=ALU.mult),
                         reads=["ps2", "rS"], writes=["t2_%d" % u])
                    S.op("dve", lambda: nc.vector.tensor_tensor(out=q[:], in0=t1[u][:], in1=t2[u][:], op=ALU.add),
                         reads=["t1_%d" % u, "t2_%d" % u], writes=[qn])
                if kind == "q":
                    dst = qT.ap()[dest + hh, :, tok0 - OWN:tok0 - OWN + TT]
                else:
                    dst = kT.ap()[dest + hh, :, tok0:tok0 + TT]
                S.dma("sp", qn, dst, q[:], reads=[qn], writes=["dram_qk"])

            def post_v(bank, bn, vcol, tb):
                s = qi[0] % 4
                qi[0] += 1
                q = qsb[s]
                qn = "qsb%d" % s
                S.op("act", lambda: nc.scalar.copy(out=q[:], in_=bank[:]), reads=[bn], writes=[qn])
                r0 = tok0 + tb * 128
                S.dma("sp", qn, vv.ap()[r0:r0 + 128, vcol:vcol + 512], q[:], reads=[qn], writes=["dram_v"])

            def post_g(bank, bn):
                S.op("act", lambda: nc.scalar.activation(out=gsb[:], in_=bank[:], func=AF.Sigmoid),
                     reads=[bn], writes=["gsb"])
                S.dma("sp", "gsb", gTd.ap()[:, tok0 - OWN:tok0 - OWN + TT], gsb[0:48, :], reads=["gsb"], writes=["dram_g"])

            pending = None
            KD = int(os.environ.get("KDBG", "0"))
            for bi, (c0, ncol, kind, dest, rope, own_only) in enumerate(blocks):
                if own_only and not own:
                    continue
                if KD == 1 or (KD >= 2 and bi not in (0, 8, 22)[:KD - 1]):
                    continue
                w = Wt[wi[0] % 2]
                wn = "WtA%d" % (wi[0] % 2)
                wi[0] += 1
                if kind == "g":
                    gsrc_ = w_in.ap()[:, c0:c0 + ncol].rearrange("(kc p) c -> p kc c", p=128)
                    for k4 in range(8):
                        S.dma("pool", "WtG", w[:, 4 * k4:4 * k4 + 4, 0:ncol], gsrc_[:, 4 * k4:4 * k4 + 4, :], writes=[wn])
                elif bi not in cast_done:
                    cast_done.add(bi)
                    mk_cast(wb_in.ap()[bi].rearrange("p (kc c) -> p kc c", kc=32)[:, :, 0:ncol],
                            w_in.ap()[:, c0:c0 + ncol].rearrange("(kc p) c -> p kc c", p=128), "wbin%d" % bi, 8, 32)()
                elif cast_q:
                    cast_q.pop(0)()
                wsrc = wb_in.ap()[bi].rearrange("p (kc c) -> p kc c", kc=32)
                for k4 in range(4 if kind != "g" else 0):
                    S.dma("sp", wn, w[:, 8 * k4:8 * k4 + 8, 0:ncol], wsrc[:, 8 * k4:8 * k4 + 8, 0:ncol], reads=["wbin%d" % bi], writes=[wn])
                nunits = 1 if kind == "g" else 4
                for un in range(nunits):
                    b = acci[0] % 4
                    acci[0] += 1
                    bank = acc[b]
                    bn = "accA%d" % b
                    for kc in range(32):
                        if kind in ("q", "k"):
                            S.op("pe", lambda kc=kc, un=un: nc.tensor.matmul(bank[:], lhsT=w[:, kc, un * 128:(un + 1) * 128],
                                                                        rhs=hT[:, kc, :], start=(kc == 0), stop=(kc == 31)),
                                 reads=[wn, "hT"], writes=[bn])
                        elif kind == "v":
                            S.op("pe", lambda kc=kc, un=un: nc.tensor.matmul(bank[:], lhsT=hT[:, kc, un * 128:(un + 1) * 128],
                                                                        rhs=w[:, kc, :], start=(kc == 0), stop=(kc == 31)),
                                 reads=[wn, "hT"], writes=[bn])
                        else:
                            S.op("pe", lambda kc=kc: nc.tensor.matmul(bank[:], lhsT=w[:, kc, 0:128],
                                                                 rhs=hT[:, kc, :], start=(kc == 0), stop=(kc == 31)),
                                 reads=[wn, "hT"], writes=[bn])
                    if pending is not None:
                        pending()
                    if kind in ("q", "k"):
                        pending = (lambda bank=bank, bn=bn, kind=kind, dest=dest, rope=rope, un=un:
                                   post_fm(bank, bn, kind, dest, rope, un))
                    elif kind == "v":
                        pending = (lambda bank=bank, bn=bn, dest=dest, un=un: post_v(bank, bn, dest, un))
                    else:
                        pending = (lambda bank=bank, bn=bn: post_g(bank, bn))
            if pending is not None:
                pending()
            if tile == list(tiles)[0]:
                pre = []
                for bi2, (c02, ncol2, _k, _d, _r, _o) in enumerate(blocks):
                    if bi2 not in cast_done and _k != "g":
                        cast_done.add(bi2)
                        pre.append(mk_cast(wb_in.ap()[bi2].rearrange("p (kc c) -> p kc c", kc=32)[:, :, 0:ncol2],
                                           w_in.ap()[:, c02:c02 + ncol2].rearrange("(kc p) c -> p kc c", p=128), "wbin%d" % bi2, 8, 32))
                cast_q[0:0] = pre
        while cast_q:
            cast_q.pop(0)()
        for j in cast_d:
            j()
        S.barrier()
    if stop_after == "A":
        return finish(nc, S, stack, y)


    if not skip_attn:
        attention_phases(nc, S, stack, sb, ps, dict(
            qT=qT, kT=kT, vv=vv, gTd=gTd, oT=oT, cw1=cw1, cw2=cw2, cpeT=cpeT, mA=mA_d, mW=mW_d, mC=mC_d,
            cmask=cmask_d, span=span_d, VM=VM_d, FB=FB_d, eexp=eexp_d, kval=kval_d, ones=ones, ident=ident),
            tiles_c)
    else:
        with ExitStack() as pz:
            zt = sb("zeroT", [128, OWN], BF16, pz)
            S.op("dve", lambda: nc.vector.memset(zt[:], 0.0), writes=["zt"])
            for h in range(32):
                S.dma("sp", "zt", oT.ap()[h, :, :], zt[:], reads=["zt"], writes=["dram_oT"])
            S.barrier()
    gout = sb("gout", [128, 32], F32)
    S.dma("sp", "c2", gout[:], g_outT.ap(), writes=["gout"])
    for t in tiles_d:
        q0 = t * TT
        with ExitStack() as pt:
          h2T = sb("h2T", [128, 32, TT], BF16, pt)
          with ExitStack() as pdx:
           x1 = sb("x1D", [128, 4, DM], F32, pdx)
           with ExitStack() as pd:
            OT = sb("OT", [128, 32, TT], BF16, pd)
            sq = [sb("sqD%d" % i, [128, 8, TT], BF16, pd) for i in range(2)]
            rs = [sb("rsD%d" % i, [128, TT], F32, pd) for i in range(2)]
            Wo = [sb("WoD%d" % i, [128, 32, 256], BF16, pd) for i in range(2)]
            xr = [sb("xrD%d" % i, [128, 4, 256], F32, pd) for i in range(2)]
            ssp = [ps("sspD%d" % i, [128, TT], F32, pd) for i in range(2)]
            acc = [ps("accD%d" % i, [128, 256], F32, pd) for i in range(4)]
            S.dma("sp", "OT", OT[:], oT.ap()[:, :, q0:q0 + TT].rearrange("h d t -> d h t"), reads=["dram_oT"], writes=["OT"])
            for g in range(2):
                for i8 in range(2):
                    u = (g * 2 + i8) % 2
                    h0 = g * 16 + i8 * 8
                    S.op("act", lambda u=u, h0=h0: nc.scalar.activation(out=sq[u][:], in_=OT[:, h0:h0 + 8, :], func=AF.Square),
                         reads=["OT"], writes=["sq%d" % u])
                    for j in range(8):
                        S.op("pe", lambda u=u, j=j, g=g, i8=i8: nc.tensor.matmul(ssp[g][:], lhsT=ones[:], rhs=sq[u][:, j, :],
                                                                          start=(i8 == 0 and j == 0), stop=(i8 == 1 and j == 7)),
                             reads=["sq%d" % u, "ones"], writes=["ssp%d" % g])
                S.op("act", lambda g=g: nc.scalar.activation(out=rs[g][:], in_=ssp[g][:], func=AF.Sqrt, scale=1.0 / 2048.0, bias=epsb[:]),
                     reads=["ssp%d" % g, "epsb"], writes=["rs%d" % g])
                S.op("dve", lambda g=g: nc.vector.reciprocal(out=rs[g][:], in_=rs[g][:]), reads=["rs%d" % g], writes=["rs%d" % g])
            for h in range(32):
                S.op("dve", lambda h=h: nc.vector.scalar_tensor_tensor(out=OT[:, h, :], in0=OT[:, h, :], scalar=gout[:, h:h + 1],
                                                                  in1=rs[h // 16][:], op0=ALU.mult, op1=ALU.mult),
                     reads=["OT", "gout", "rs%d" % (h // 16)], writes=["OT"])
            for cb in range(16):
                w = Wo[cb % 2]
                wn = "WoD%d" % (cb % 2)
                c0 = cb * 256
                wsrc = wb_o.ap()[cb].rearrange("p (h c) -> p h c", h=32)
                for k4 in range(2):
                    S.dma("sp", wn, w[:, 16 * k4:16 * k4 + 16, :], wsrc[:, 16 * k4:16 * k4 + 16, :], reads=["wbo%d" % cb], writes=[wn])
                xrt = xr[cb % 2]
                xn = "xrD%d" % (cb % 2)
                S.dma("sp", xn, xrt[:], xc.ap()[OWN + q0:OWN + q0 + TT, c0:c0 + 256].rearrange("(b p) c -> p b c", p=128),
                      writes=[xn])
                for tb in range(4):
                    for h in range(32):
                        S.op("pe", lambda tb=tb, h=h: nc.tensor.matmul(acc[tb][:], lhsT=OT[:, h, tb * 128:(tb + 1) * 128], rhs=w[:, h, :],
                                                                  start=(h == 0), stop=(h == 31)),
                             reads=["OT", wn], writes=["accD%d" % tb])
                    S.op("dve", lambda tb=tb: nc.vector.tensor_tensor(out=x1[:, tb, c0:c0 + 256], in0=acc[tb][:], in1=xrt[:, tb, :], op=ALU.add),
                         reads=["accD%d" % tb, xn], writes=["x1"])
            S.dma("sp", "x1o", x1d.ap()[q0:q0 + TT, :].rearrange("(b p) c -> p b c", p=128), x1[:], reads=["x1"], writes=["dram_x1"])
            S.barrier()
           if True:
            with ExitStack() as pd3:
                gainF = sb("gainF", [128, DM], F32, pd3)
                xs = sb("xsD", [128, DM], BF16, pd3)
                ssq = sb("ssqD", [128, 1], F32, pd3)
                rstd = sb("rstdD", [128, 1], F32, pd3)
                tp = [ps("tpD%d" % i, [128, 1024], BF16, pd3) for i in range(2)]
                S.dma("sp", "gain", gainF[:], dap(g_ffn, 0, [[0, 128], [1, DM]]), writes=["gain"])
                tpi = [0]
                for tb in range(4):
                    norm_transpose("x1", x1[:, tb, :], gainF, xs, h2T, tb, tp, ssq, rstd, tpi)
                S.barrier()
          ffn_tile(nc, S, stack, sb, ps, t, q0, h2T, wb_g, wb_u, wb_d, x1d, g_fin, y, epsb)
          S.barrier()
    return finish(nc, S, stack, y)


def attention_phases(nc, S, stack, sb, ps, T, tiles_c):
    qT, kT, vv, gTd, oT = T["qT"], T["kT"], T["vv"], T["gTd"], T["oT"]
    ones, ident = T["ones"], T["ident"]
    EXP = AF.Exp
    kval = sb("kval", [128, 2, 128], BF16)
    S.dma("sp", "c3", kval[:], T["kval"].ap(), writes=["kval"])
    kcT_all = sb("kcT_all", [128, 4, 256], BF16)
    vc_all = sb("vc_all", [128, 4, 2, 128], BF16)

    with ExitStack() as pb:
        XT = sb("XTB", [128, CTX + 32], BF16, pb)
        W1 = sb("W1B", [128, 32, 128], BF16, pb)
        W2 = sb("W2B", [128, 128], BF16, pb)
        pe32 = sb("pe32", [128, 32], F32, pb)
        pe16 = sb("pe16", [128, 32], BF16, pb)
        b1 = sb("b1B", [128, 1], F32, pb)
        xg = sb("xgB", [128, 256], F32, pb)
        x2 = sb("x2B", [128, 256], F32, pb)
        gTb = sb("gTB", [128, 256], BF16, pb)
        pb1 = ps("pb1", [128, 512], F32, pb)
        po1 = ps("po1", [128, 512], F32, pb)
        po2 = ps("po2", [128, 512], F32, pb)
        S.op("dve", lambda: nc.vector.memset(XT[:, CTX:CTX + 32], 0.0), writes=["XT"])
        for kind in range(2):
            S.dma("pool", "W1B", W1[:], T["cw1"][kind].ap().rearrange("(i d) h -> d i h", d=128), writes=["W1"])
            S.dma("pool", "W2B", W2[:], T["cw2"][kind].ap(), writes=["W2"])
            S.dma("sp", "pe32", pe32[:], T["cpeT"][kind].ap(), writes=["pe32"])
            S.op("dve", lambda: nc.vector.tensor_copy(out=pe16[:], in_=pe32[:]), reads=["pe32"], writes=["pe16"])
            for i in range(32):
                S.op("pe", lambda i=i: nc.tensor.matmul(pb1[:, 0:1], lhsT=W1[:, i, :], rhs=pe16[:, i:i + 1], start=(i == 0), stop=(i == 31)),
                     reads=["W1", "pe16"], writes=["pb1"])
            S.op("dve", lambda: nc.vector.tensor_copy(out=b1[:], in_=pb1[:, 0:1]), reads=["pb1"], writes=["b1"])
            for g in range(4):
                slot = (KT_KBC if kind == 0 else KT_VBC) + g
                S.dma("sp", "XTB", XT[:, 0:CTX], kT.ap()[slot, :, :], reads=["dram_qk"], writes=["XT"])
                for i in range(32):
                    rhs = dap(XT, i, [[CTX + 32, 128], [16, 256]])
                    S.op("pe", lambda i=i, rhs=rhs: nc.tensor.matmul(po1[:, 0:256], lhsT=W1[:, i, :], rhs=rhs, start=(i == 0), stop=(i == 31)),
                         reads=["W1", "XT"], writes=["po1"])
                S.op("dve", lambda: nc.vector.tensor_scalar(out=xg[:], in0=po1[:, 0:256], scalar1=b1[:, 0:1], scalar2=None, op0=ALU.add),
                     reads=["po1", "b1"], writes=["xg"])
                S.op("act", lambda: nc.scalar.activation(out=x2[:], in_=xg[:], func=AF.Square), reads=["xg"], writes=["x2"])
                S.op("dve", lambda: nc.vector.tensor_scalar(out=x2[:], in0=x2[:], scalar1=0.044715, scalar2=1.0, op0=ALU.mult, op1=ALU.add),
                     reads=["x2"], writes=["x2"])
                S.op("dve", lambda: nc.vector.tensor_tensor(out=x2[:], in0=x2[:], in1=xg[:], op=ALU.mult), reads=["x2", "xg"], writes=["x2"])
                S.op("act", lambda: nc.scalar.activation(out=x2[:], in_=x2[:], func=AF.Sigmoid, scale=1.5957691216057308),
                     reads=["x2"], writes=["x2"])
                S.op("dve", lambda: nc.vector.tensor_tensor(out=gTb[:], in0=x2[:], in1=xg[:], op=ALU.mult), reads=["x2", "xg"], writes=["gTb"])
                if kind == 0:
                    S.op("pe", lambda: nc.tensor.matmul(po2[:, 0:256], lhsT=W2[:], rhs=gTb[:], start=True, stop=True),
                         reads=["W2", "gTb"], writes=["po2"])
                    S.op("act", lambda g=g: nc.scalar.copy(out=kcT_all[:, g, :], in_=po2[:, 0:256]), reads=["po2"], writes=["kcT_all"])
                else:
                    for cb in range(2):
                        S.op("pe", lambda cb=cb: nc.tensor.matmul(po2[:, cb * 128:(cb + 1) * 128], lhsT=gTb[:, cb * 128:(cb + 1) * 128], rhs=W2[:],
                                                             start=True, stop=True), reads=["W2", "gTb"], writes=["po2"])
                    S.op("act", lambda g=g: nc.scalar.copy(out=vc_all[:, g, :, :], in_=po2[:, 0:256].rearrange("p (a b) -> p a b", a=2)),
                         reads=["po2"], writes=["vc_all"])
        S.barrier()

    def pair(ST, stn, lhsK, rhsQ, E, en, PT, pn, mask_ap, mreads, O, on, lhsV, vreads, Dn, dn, lhsD, dreads, first, last):
        S.op("pe", lambda: nc.tensor.matmul(ST[:], lhsT=lhsK, rhs=rhsQ, start=True, stop=True), reads=vreads[:1] + ["QT"], writes=[stn])
        S.op("act", lambda: nc.scalar.activation(out=E[:], in_=ST[:], func=EXP, scale=SCALE), reads=[stn], writes=[en])
        S.op("dve", lambda: nc.vector.tensor_tensor(out=PT[:], in0=E[:], in1=mask_ap, op=ALU.mult), reads=[en] + mreads, writes=[pn])
        S.op("pe", lambda: nc.tensor.matmul(O[:], lhsT=lhsV, rhs=PT[:], start=first, stop=last), reads=[pn] + vreads[1:], writes=[on])
        S.op("pe", lambda: nc.tensor.matmul(Dn[:], lhsT=lhsD, rhs=PT[:], start=first, stop=last), reads=[pn] + dreads, writes=[dn])

    with ExitStack() as pc:
        mA = sb("mA", [128, 3072], BF16, pc)
        S.dma("sp", "c4", mA[:], T["mA"].ap(), writes=["mA"])
        KT = [sb("KTA%d" % i, [128, CTX], BF16, pc) for i in range(2)]
        VH = [sb("VHA%d" % i, [128, 32, 128], BF16, pc) for i in range(2)]
        QT = [sb("QTA%d" % i, [128, OWN], BF16, pc) for i in range(2)]
        E = [sb("EA%d" % i, [128, TT], BF16, pc) for i in range(3)]
        PT = [sb("PTA%d" % i, [128, TT], BF16, pc) for i in range(3)]
        rden = [sb("rdA%d" % i, [128, TT], F32, pc) for i in range(2)]
        ob = [sb("obA%d" % i, [128, TT], BF16, pc) for i in range(2)]
        ST = [ps("STA%d" % i, [128, TT], F32, pc) for i in range(3)]
        O = [ps("OA%d" % i, [128, TT], F32, pc) for i in range(2)]
        Dn = [ps("DA%d" % i, [128, TT], F32, pc) for i in range(2)]
        pi = 0
        oi = 0
        for h in range(16):
            u = h % 2
            S.dma("sp", "KTA%d" % u, KT[u][:], kT.ap()[KT_KA + h, :, :], reads=["dram_qk"], writes=["KT%d" % u])
            S.dma("sp", "VHA%d" % u, VH[u][:], vv.ap()[:, V_VA + h * 128:V_VA + (h + 1) * 128].rearrange("(kb p) d -> p kb d", p=128),
                  reads=["dram_v"], writes=["VH%d" % u])
            S.dma("sp", "QTA%d" % u, QT[u][:], qT.ap()[h, :, :], reads=["dram_qk"], writes=["QT%d" % u])
            for t in tiles_c:
                o = oi % 2
                oi += 1
                kbs = list(range(4 * t, 4 * t + 20))
                for n, kb in enumerate(kbs):
                    D = 16 + 4 * t - kb
                    s3 = pi % 3
                    pi += 1
                    S.op("pe", lambda kb=kb, s3=s3: nc.tensor.matmul(ST[s3][:], lhsT=KT[u][:, kb * 128:(kb + 1) * 128],
                                                                 rhs=QT[u][:, t * TT:(t + 1) * TT], start=True, stop=True),
                         reads=["KT%d" % u, "QT%d" % u], writes=["STA%d" % s3])
                    S.op("act", lambda s3=s3: nc.scalar.activation(out=E[s3][:], in_=ST[s3][:], func=EXP, scale=SCALE),
                         reads=["STA%d" % s3], writes=["EA%d" % s3])
                    S.op("dve", lambda s3=s3, D=D: nc.vector.tensor_tensor(out=PT[s3][:], in0=E[s3][:], in1=mA[:, 128 * D + 384:128 * D + 896], op=ALU.mult),
                         reads=["EA%d" % s3, "mA"], writes=["PTA%d" % s3])
                    S.op("pe", lambda kb=kb, s3=s3, n=n: nc.tensor.matmul(O[o][:], lhsT=VH[u][:, kb, :], rhs=PT[s3][:], start=(n == 0), stop=(n == 19)),
                         reads=["PTA%d" % s3, "VH%d" % u], writes=["OA%d" % o])
                    S.op("pe", lambda kb=kb, s3=s3, n=n: nc.tensor.matmul(Dn[o][:], lhsT=kval[:, 0 if kb < 16 else 1, :], rhs=PT[s3][:],
                                                                      start=(n == 0), stop=(n == 19)),
                         reads=["PTA%d" % s3, "kval"], writes=["DA%d" % o])
                S.op("dve", lambda o=o: nc.vector.reciprocal(out=rden[o][:], in_=Dn[o][:]), reads=["DA%d" % o], writes=["rdA%d" % o])
                S.op("dve", lambda o=o: nc.vector.tensor_tensor(out=ob[o][:], in0=O[o][:], in1=rden[o][:], op=ALU.mult),
                     reads=["OA%d" % o, "rdA%d" % o], writes=["obA%d" % o])
                S.dma("sp", "obA%d" % o, oT.ap()[h, :, t * TT:(t + 1) * TT], ob[o][:], reads=["obA%d" % o], writes=["dram_oT"])
        S.barrier()

    with ExitStack() as pn_:
        st = pn_
        mW = sb("mW", [128, 1408], BF16, st)
        mC = sb("mC", [128, 1024], BF16, st)
        cmask = sb("cmask", [128, 2, OWN], BF16, st)
        span = sb("span", [128, 2, 64], BF16, st)
        VM = sb("VM", [128, 16, 64], F32, st)
        FB = sb("FB", [128, 16, 64], F32, st)
        eexp = sb("eexp", [128, 32, 128], BF16, st)
        for nm, tl in (("mW", mW), ("mC", mC), ("cmask", cmask), ("span", span), ("VM", VM), ("FB", FB), ("eexp", eexp)):
            S.dma("sp", "k_" + nm, tl[:], T[nm].ap(), writes=[nm])
        QB = sb("QB", [128, 4, OWN], BF16, st)
        KS = sb("KS", [128, CTX], BF16, st)
        VS = sb("VS", [128, 32, 128], BF16, st)
        KW = sb("KW", [128, CTX], BF16, st)
        VW = sb("VW", [128, 32, 128], BF16, st)
        EC = sb("EC", [128, 4, 2, TT], BF16, st)
        SM = sb("SM", [128, 32, TT], BF16, st)
        accO = [sb("accO%d" % i, [128, TT], F32, st) for i in range(4)]
        G = [sb("G%d" % i, [128, 3, TT], F32, st) for i in range(4)]
        E = [sb("EB%d" % i, [128, TT], BF16, st) for i in range(3)]
        PT = [sb("PTB%d" % i, [128, TT], BF16, st) for i in range(3)]
        rcb = sb("rcb", [128, TT], F32, st)
        coef = sb("coef", [128, TT], F32, st)
        tmpo = sb("tmpo", [128, TT], F32, st)
        sc = sb("sc", [128, 4, 64], F32, st)
        sc2 = sb("sc2", [128, 64], F32, st)
        m8 = sb("m8", [128, 8], F32, st)
        selp = sb("selp", [128, 4, 128], BF16, st)
        self_ = sb("self", [128, 64], F32, st)
        selT = sb("selT", [128, TT], BF16, st)
        obb = [sb("obB%d" % i, [128, TT], BF16, st) for i in range(2)]
        ST = [ps("STB%d" % i, [128, TT], F32, st) for i in range(2)]
        O = ps("OB", [128, TT], F32, st)
        Dn = ps("DB", [128, TT], F32, st)
        IMP = ps("IMP", [128, TT], F32, st)
        TPS = ps("TPS", [128, 1024], BF16, st)
        MB = [ps("MB%d" % i, [128, TT], F32, st) for i in range(2)]
        S.op("dve", lambda: nc.vector.memset(selp[:], 0.0), writes=["selp"])
        pi = 0
        obi = 0

        def branch(r, t, Ksb, kname, Vsb, vname, kbs, mask_of, mreads, den_of, dreads):
            nonlocal pi
            for n, kb in enumerate(kbs):
                s2 = pi % 2
                s3 = pi % 3
                pi += 1
                S.op("pe", lambda: nc.tensor.matmul(ST[s2][:], lhsT=Ksb[:, kb * 128:(kb + 1) * 128], rhs=QB[:, r, t * TT:(t + 1) * TT],
                                                   start=True, stop=True), reads=[kname, "QB"], writes=["STB%d" % s2])
                S.op("act", lambda: nc.scalar.activation(out=E[s3][:], in_=ST[s2][:], func=EXP, scale=SCALE),
                     reads=["STB%d" % s2], writes=["EB%d" % s3])
                S.op("dve", lambda: nc.vector.tensor_tensor(out=PT[s3][:], in0=E[s3][:], in1=mask_of(kb), op=ALU.mult),
                     reads=["EB%d" % s3] + mreads, writes=["PTB%d" % s3])
                S.op("pe", lambda: nc.tensor.matmul(O[:], lhsT=Vsb[:, kb, :], rhs=PT[s3][:], start=(n == 0), stop=(n == len(kbs) - 1)),
                     reads=["PTB%d" % s3, vname], writes=["OB"])
                S.op("pe", lambda: nc.tensor.matmul(Dn[:], lhsT=den_of(kb), rhs=PT[s3][:], start=(n == 0), stop=(n == len(kbs) - 1)),
                     reads=["PTB%d" % s3] + dreads, writes=["DB"])

        def combine(r, br, first):
            S.op("dve", lambda: nc.vector.reciprocal(out=rcb[:], in_=Dn[:]), reads=["DB"], writes=["rcb"])
            S.op("dve", lambda: nc.vector.tensor_tensor(out=coef[:], in0=rcb[:], in1=G[r][:, br, :], op=ALU.mult),
                 reads=["rcb", "G%d" % r], writes=["coef"])
            if first:
                S.op("dve", lambda: nc.vector.tensor_tensor(out=accO[r][:], in0=O[:], in1=coef[:], op=ALU.mult),
                     reads=["OB", "coef"], writes=["accO%d" % r])
            else:
                S.op("dve", lambda: nc.vector.tensor_tensor(out=tmpo[:], in0=O[:], in1=coef[:], op=ALU.mult),
                     reads=["OB", "coef"], writes=["tmpo"])
                S.op("dve", lambda: nc.vector.tensor_tensor(out=accO[r][:], in0=accO[r][:], in1=tmpo[:], op=ALU.add),
                     reads=["tmpo", "accO%d" % r], writes=["accO%d" % r])

        for g in range(4):
            for r in range(4):
                S.dma("sp", "QB", QB[:, r, :], qT.ap()[16 + 4 * g + r, :, :], reads=["dram_qk"], writes=["QB"])
            S.dma("sp", "KS", KS[:], kT.ap()[KT_KBS + g, :, :], reads=["dram_qk"], writes=["KS"])
            S.dma("sp", "KW", KW[:], kT.ap()[KT_KBW + g, :, :], reads=["dram_qk"], writes=["KW"])
            S.dma("sp", "VS", VS[:], vv.ap()[:, V_VBS + g * 128:V_VBS + (g + 1) * 128].rearrange("(kb p) d -> p kb d", p=128),
                  reads=["dram_v"], writes=["VS"])
            S.dma("sp", "VW", VW[:], vv.ap()[:, V_VBW + g * 128:V_VBW + (g + 1) * 128].rearrange("(kb p) d -> p kb d", p=128),
                  reads=["dram_v"], writes=["VW"])
            for t in tiles_c:
                for r in range(4):
                    S.dma("sp", "G%d" % r, G[r][:], dap(gTd, ((4 * g + r) * 3) * OWN + t * TT, [[0, 128], [OWN, 3], [1, TT]]),
                          reads=["dram_g"], writes=["G%d" % r])
                    for cb in range(2):
                        s2 = pi % 2
                        s3 = pi % 3
                        pi += 1
                        S.op("pe", lambda cb=cb, s2=s2: nc.tensor.matmul(ST[s2][:], lhsT=kcT_all[:, g, cb * 128:(cb + 1) * 128],
                                                                     rhs=QB[:, r, t * TT:(t + 1) * TT], start=True, stop=True),
                             reads=["kcT_all", "QB"], writes=["STB%d" % s2])
                        S.op("act", lambda s2=s2, s3=s3: nc.scalar.activation(out=E[s3][:], in_=ST[s2][:], func=EXP, scale=SCALE),
                             reads=["STB%d" % s2], writes=["EB%d" % s3])
                        S.op("dve", lambda cb=cb, s3=s3: nc.vector.tensor_tensor(out=EC[:, r, cb, :], in0=E[s3][:], in1=cmask[:, cb, t * TT:(t + 1) * TT],
                                                                            op=ALU.mult), reads=["EB%d" % s3, "cmask"], writes=["EC"])
                        S.op("pe", lambda cb=cb: nc.tensor.matmul(O[:], lhsT=vc_all[:, g, cb, :], rhs=EC[:, r, cb, :], start=(cb == 0), stop=(cb == 1)),
                             reads=["EC", "vc_all"], writes=["OB"])
                        S.op("pe", lambda cb=cb: nc.tensor.matmul(Dn[:], lhsT=ones[:], rhs=EC[:, r, cb, :], start=(cb == 0), stop=(cb == 1)),
                             reads=["EC", "ones"], writes=["DB"])
                    S.op("dve", lambda: nc.vector.tensor_scalar(out=rcb[:], in0=Dn[:], scalar1=1e-30, scalar2=None, op0=ALU.max),
                         reads=["DB"], writes=["rcb"])
                    S.op("dve", lambda: nc.vector.reciprocal(out=rcb[:], in_=rcb[:]), reads=["rcb"], writes=["rcb"])
                    S.op("dve", lambda: nc.vector.tensor_tensor(out=coef[:], in0=rcb[:], in1=G[r][:, 0, :], op=ALU.mult),
                         reads=["rcb", "G%d" % r], writes=["coef"])
                    S.op("dve", lambda: nc.vector.tensor_tensor(out=accO[r][:], in0=O[:], in1=coef[:], op=ALU.mult),
                         reads=["OB", "coef"], writes=["accO%d" % r])
                    for cb in range(2):
                        S.op("dve", lambda cb=cb: nc.vector.tensor_tensor(out=EC[:, r, cb, :], in0=EC[:, r, cb, :], in1=rcb[:], op=ALU.mult),
                             reads=["EC", "rcb"], writes=["EC"])
                for qb in range(4):
                    n = 0
                    for r in range(4):
                        for cb in range(2):
                            S.op("pe", lambda qb=qb, r=r, cb=cb, n=n: nc.tensor.matmul(IMP[:, qb * 64:(qb + 1) * 64], lhsT=EC[:, r, cb, qb * 128:(qb + 1) * 128],
                                                                               rhs=span[:, cb, :], start=(n == 0), stop=(n == 7)),
                                 reads=["EC", "span"], writes=["IMP"])
                            n += 1
                S.op("dve", lambda: nc.vector.tensor_tensor(out=sc[:], in0=IMP[:, 0:256].rearrange("p (a b) -> p a b", a=4),
                                                            in1=VM[:, 4 * t:4 * t + 4, :], op=ALU.mult), reads=["IMP", "VM"], writes=["sc"])
                S.op("dve", lambda: nc.vector.tensor_tensor(out=sc[:], in0=sc[:], in1=FB[:, 4 * t:4 * t + 4, :], op=ALU.add),
                     reads=["sc", "FB"], writes=["sc"])
                for qb in range(4):
                    S.op("dve", lambda qb=qb: nc.vector.max(out=m8[:], in_=sc[:, qb, :]), reads=["sc"], writes=["m8"])
                    S.op("dve", lambda qb=qb: nc.vector.match_replace(out=sc2[:], in_to_replace=m8[:], in_values=sc[:, qb, :], imm_value=-3.0e38),
                         reads=["sc", "m8"], writes=["sc2"])
                    S.op("dve", lambda: nc.vector.max(out=m8[:], in_=sc2[:]), reads=["sc2"], writes=["m8"])
                    S.op("dve", lambda qb=qb: nc.vector.tensor_scalar(out=self_[:], in0=sc[:, qb, :], scalar1=m8[:, 7:8], scalar2=None, op0=ALU.is_ge),
                         reads=["sc", "m8"], writes=["self"])
                    S.op("dve", lambda qb=qb: nc.vector.tensor_tensor(out=selp[:, qb, 0:64], in0=self_[:], in1=VM[:, 4 * t + qb, :], op=ALU.mult),
                         reads=["self", "VM"], writes=["selp"])
                for qb in range(4):
                    S.op("pe", lambda qb=qb: nc.tensor.transpose(out=TPS[:, qb * 128:(qb + 1) * 128], in_=selp[:, qb, :], identity=ident[:]),
                         reads=["selp", "ident"], writes=["TPS"])
                S.op("act", lambda: nc.scalar.copy(out=selT[:], in_=TPS[:, 0:TT]), reads=["TPS"], writes=["selT"])
                nkb = 20 + 4 * t
                for kb in range(nkb):
                    D = min(16 + 4 * t - kb, 1)
                    mb = kb % 2
                    S.op("pe", lambda kb=kb, mb=mb: nc.tensor.matmul(MB[mb][:], lhsT=eexp[:, kb, :], rhs=selT[:], start=True, stop=True),
                         reads=["eexp", "selT"], writes=["MB%d" % mb])
                    S.op("dve", lambda kb=kb, mb=mb, D=D: nc.vector.tensor_tensor(out=SM[:, kb, :], in0=MB[mb][:], in1=mC[:, 128 * D + 384:128 * D + 896],
                                                                             op=ALU.mult), reads=["MB%d" % mb, "mC"], writes=["SM"])
                for r in range(4):
                    branch(r, t, KS, "KS", VS, "VS", list(range(nkb)), lambda kb: SM[:, kb, :], ["SM"], lambda kb: ones[:], ["ones"])
                    combine(r, 1, False)
                    branch(r, t, KW, "KW", VW, "VW", list(range(4 * t + 12, 4 * t + 20)),
                           lambda kb: mW[:, 128 * (16 + 4 * t - kb) + 384:128 * (16 + 4 * t - kb) + 896], ["mW"],
                           lambda kb: kval[:, 0 if kb < 16 else 1, :], ["kval"])
                    combine(r, 2, False)
                    o = obi % 2
                    obi += 1
                    S.op("act", lambda o=o, r=r: nc.scalar.copy(out=obb[o][:], in_=accO[r][:]), reads=["accO%d" % r], writes=["obB%d" % o])
                    S.dma("sp", "obB%d" % o, oT.ap()[16 + 4 * g + r, :, t * TT:(t + 1) * TT], obb[o][:], reads=["obB%d" % o], writes=["dram_oT"])
        S.barrier()


def ffn_tile(nc, S, stack, sb, ps, t, q0, h2T, wb_g, wb_u, wb_d, x1d, g_fin, y, epsb):
    with ExitStack() as p4:
        actT = sb("actT", [128, 86, TT], BF16, p4)
        with ExitStack() as p4a:
            Wg = [sb("WgD%d" % i, [128, 32, 256], BF16, p4a) for i in range(2)]
            Wu = [sb("WuD%d" % i, [128, 32, 256], BF16, p4a) for i in range(2)]
            sl = [sb("slD%d" % i, [128, TT], F32, p4a) for i in range(2)]
            gb = [ps("gbD%d" % i, [128, TT], F32, p4a) for i in range(2)]
            ub = [ps("ubD%d" % i, [128, TT], F32, p4a) for i in range(2)]
            for fb in range(43):
                wg, wu = Wg[fb % 2], Wu[fb % 2]
                gn, un_ = "WgD%d" % (fb % 2), "WuD%d" % (fb % 2)
                f0 = fb * 256
                gsrc = wb_g.ap()[fb].rearrange("p (h c) -> p h c", h=32)
                usrc = wb_u.ap()[fb].rearrange("p (h c) -> p h c", h=32)
                for k4 in range(2):
                    S.dma("sp", gn, wg[:, 16 * k4:16 * k4 + 16, :], gsrc[:, 16 * k4:16 * k4 + 16, :], reads=["wbg%d" % fb], writes=[gn])
                    S.dma("sp", un_, wu[:, 16 * k4:16 * k4 + 16, :], usrc[:, 16 * k4:16 * k4 + 16, :], reads=["wbu%d" % fb], writes=[un_])
                for j in range(2):
                    fc = fb * 2 + j
                    b = fc % 2
                    for kc in range(32):
                        S.op("pe", lambda kc=kc: nc.tensor.matmul(gb[b][:], lhsT=wg[:, kc, j * 128:(j + 1) * 128], rhs=h2T[:, kc, :],
                                                             start=(kc == 0), stop=(kc == 31)), reads=[gn, "hT"], writes=["gb%d" % b])
                    for kc in range(32):
                        S.op("pe", lambda kc=kc: nc.tensor.matmul(ub[b][:], lhsT=wu[:, kc, j * 128:(j + 1) * 128], rhs=h2T[:, kc, :],
                                                             start=(kc == 0), stop=(kc == 31)), reads=[un_, "hT"], writes=["ub%d" % b])
                    S.op("act", lambda: nc.scalar.activation(out=sl[b][:], in_=gb[b][:], func=AF.Silu), reads=["gb%d" % b], writes=["sl%d" % b])
                    S.op("dve", lambda: nc.vector.tensor_tensor(out=actT[:, fc, :], in0=ub[b][:], in1=sl[b][:], op=ALU.mult),
                         reads=["ub%d" % b, "sl%d" % b], writes=["actT"])
            S.barrier()
        with ExitStack() as p5:
            Wd = [sb("WdD%d" % i, [128, 4, 512], BF16, p5) for i in range(2)]
            x2 = sb("x2D", [128, 2, 4096], F32, p5)
            x1r = sb("x1rD", [128, 2, 512], F32, p5)
            gfin = sb("gfinD", [128, 4096], F32, p5)
            junk = sb("junkD", [128, 4096], BF16, p5)
            ssq = sb("ssq5", [128, 1], F32, p5)
            rstd = sb("rstd5", [128, 1], F32, p5)
            acc = [ps("acc5_%d" % i, [128, 512], F32, p5) for i in range(4)]
            S.dma("sp", "gain", gfin[:], dap(g_fin, 0, [[0, 128], [1, 4096]]), writes=["gain"])
            wi = 0
            for hf in range(2):
                r_lo = q0 + hf * 256
                for cb in range(8):
                    c0 = cb * 512
                    S.dma("sp", "x1r", x1r[:], x1d.ap()[r_lo:r_lo + 256, c0:c0 + 512].rearrange("(b p) c -> p b c", p=128),
                          reads=["dram_x1"], writes=["x1r"])
                    for f4 in range(22):
                        nf = 4 if f4 < 21 else 2
                        w = Wd[wi % 2]
                        wn = "WdD%d" % (wi % 2)
                        wi += 1
                        r0 = f4 * 4 * 128
                        wsrc = wb_d.ap()[cb, f4].rearrange("p (f c) -> p f c", f=4)[:, 0:nf, :]
                        S.dma("sp", wn, w[:, 0:nf, :], wsrc, reads=["wbd%d_%d" % (cb, f4)], writes=[wn])
                        for fl in range(nf):
                            fc = f4 * 4 + fl
                            for tb in range(2):
                                bk = (cb % 2) * 2 + tb
                                tg = hf * 2 + tb
                                S.op("pe", lambda fl=fl, fc=fc, tg=tg, bk=bk: nc.tensor.matmul(
                                    acc[bk][:], lhsT=actT[:, fc, tg * 128:(tg + 1) * 128], rhs=w[:, fl, :],
                                    start=(fc == 0), stop=(fc == 85)), reads=["actT", wn], writes=["acc5_%d" % bk])
                    for tb in range(2):
                        bk = (cb % 2) * 2 + tb
                        S.op("dve", lambda tb=tb, bk=bk: nc.vector.tensor_tensor(out=x2[:, tb, c0:c0 + 512], in0=acc[bk][:], in1=x1r[:, tb, :], op=ALU.add),
                             reads=["acc5_%d" % bk, "x1r"], writes=["x2"])
                for tb in range(2):
                    S.op("act", lambda tb=tb: nc.scalar.activation(out=junk[:], in_=x2[:, tb, :], func=AF.Square, scale=1.0 / 64.0, accum_out=ssq[:]),
                         reads=["x2"], writes=["junk", "ssq"])
                    S.op("act", lambda: nc.scalar.activation(out=ssq[:], in_=ssq[:], func=AF.Sqrt, bias=epsb[:]), reads=["ssq", "epsb"], writes=["ssq"])
                    S.op("dve", lambda: nc.vector.reciprocal(out=rstd[:], in_=ssq[:]), reads=["ssq"], writes=["rstd"])
                    S.op("dve", lambda tb=tb: nc.vector.scalar_tensor_tensor(out=x2[:, tb, :], in0=x2[:, tb, :], scalar=rstd[:, 0:1], in1=gfin[:],
                                                                        op0=ALU.mult, op1=ALU.mult), reads=["x2", "rstd", "gain"], writes=["x2"])
                S.dma("sp", "yo", y.ap()[r_lo:r_lo + 256, :].rearrange("(b p) c -> p b c", p=128), x2[:], reads=["x2"], writes=["dram_y"])
            S.barrier()


def finish(nc, S, stack, y):
    S.final_wait("sp")
    stack.close()
    return nc


def _consts(half):
    p = np.arange(128)[:, None]
    c = {}
    u = np.arange(3072)[None, :]
    d = u - 384 - p
    m = ((d >= 0) & (d <= 128)).astype(np.float32) + ((d >= 0) & (d <= 512) & (d % 4 == 0)) + \
        ((d >= 0) & (d <= 2048) & (d % 16 == 0))
    c["mA"] = m.astype(NPBF)
    u = np.arange(1408)[None, :]
    d = u - 384 - p
    c["mW"] = ((d >= 0) & (d <= 511)).astype(NPBF)
    u = np.arange(1024)[None, :]
    d = u - 384 - p
    c["mC"] = (d >= 0).astype(NPBF)
    cp = np.arange(256)
    qctx = OWN + np.arange(OWN)
    cvalid = (cp <= 254) & ((cp >= 128) | (half == 1))
    cm = ((16 * cp[:, None] + 31) <= qctx[None, :]) & cvalid[:, None]
    c["cmaskT"] = np.ascontiguousarray(cm.reshape(2, 128, OWN).transpose(1, 0, 2)).astype(NPBF)
    j = np.arange(64)
    sh = ((16 * cp[:, None]) < (64 * j[None, :] + 64)) & ((16 * cp[:, None] + 32) > 64 * j[None, :])
    c["span"] = np.ascontiguousarray(sh.reshape(2, 128, 64).transpose(1, 0, 2)).astype(NPBF)
    jabs = j[None, :] - (32 if half == 0 else 0)
    qblk_ctx = (qctx // 64)[:, None]
    j0 = 32 if half == 0 else 0
    valid = (j[None, :] * 64 <= qctx[:, None]) & (jabs >= 0)
    forced = ((j[None, :] == j0) | (j[None, :] == qblk_ctx) | (j[None, :] == qblk_ctx - 1)) & valid
    VM = valid.astype(np.float32)
    FB = np.where(forced, 1e6, np.where(valid, 0.0, -1e30)).astype(np.float32)
    c["VM"] = np.ascontiguousarray(VM.reshape(16, 128, 64).transpose(1, 0, 2))
    c["FB"] = np.ascontiguousarray(FB.reshape(16, 128, 64).transpose(1, 0, 2))
    kb = np.arange(32)
    k = np.arange(128)
    ee = (j[:, None, None] == (2 * kb[None, :, None] + (k[None, None, :] // 64)))
    c["eexp"] = np.concatenate([ee, np.zeros_like(ee)], 0).astype(NPBF)
    kv = np.ones((128, 2, 128), np.float32)
    kv[:, 0, :] = 1.0 if half == 1 else 0.0
    c["kval"] = kv.astype(NPBF)
    c["ident"] = np.eye(128, dtype=np.float32).astype(NPBF)
    r = np.zeros((128, 128), np.float32)
    for mm in range(32):
        r[(mm + 16) % 32, mm] = 1.0
    c["rm"] = r.astype(NPBF)
    pos = (np.arange(CTX) - OWN + half * OWN).astype(np.float32)
    inv = (np.float32(500000.0) ** (-np.arange(0, 32, 2, dtype=np.float32) / np.float32(32))).astype(np.float32)
    ang = pos[None, :] * inv[:, None]
    cs, sn = np.cos(ang).astype(np.float32), np.sin(ang).astype(np.float32)
    c["ropeC"] = np.concatenate([cs, cs, np.ones((96, CTX), np.float32)], 0)
    c["ropeS"] = np.concatenate([-sn, sn, np.zeros((96, CTX), np.float32)], 0)
    return c


def prep_core(inp, b, half):
    f = lambda a: np.ascontiguousarray(np.asarray(a, dtype=np.float32))
    x = np.asarray(inp["x"])
    xcv = np.zeros((CTX, DM), np.float32)
    if half == 1:
        xcv[:] = x[b]
    else:
        xcv[OWN:] = x[b, :OWN]
    m = {"xc": xcv, "w_in": f(inp["w_in"][0]), "w_out": f(inp["w_out"][0]), "w_gate": f(inp["w_gate"][0]),
         "w_up": f(inp["w_up"][0]), "w_down": f(inp["w_down"][0]),
         "ck_w1": f(inp["ck_w1"][0]), "cv_w1": f(inp["cv_w1"][0]), "ck_w2": f(inp["ck_w2"][0]),
         "cv_w2": f(inp["cv_w2"][0]), "ck_peT": f(np.asarray(inp["ck_pe"][0]).T), "cv_peT": f(np.asarray(inp["cv_pe"][0]).T),
         "g_attn": f(inp["norm_attn"][0]).reshape(1, DM), "g_ffn": f(inp["norm_ffn"][0]).reshape(1, DM),
         "g_fin": f(inp["norm_final"]).reshape(1, DM),
         "g_outT": f(np.concatenate([np.asarray(inp["out_norm_a"][0]), np.asarray(inp["out_norm_b"][0])]).reshape(32, 128).T)}
    m.update(_consts(half))
    return m


def kernel(**inputs):
    nc = build_program()
    in_maps = [prep_core(inputs, c // 2, c % 2) for c in range(8)]
    res = run_bass_kernel_spmd(nc, in_maps, core_ids=list(range(8)))
    out = np.zeros((4, SEQ, DM), np.float32)
    for c in range(8):
        out[c // 2, (c % 2) * OWN:(c % 2 + 1) * OWN] = res.results[c]["y"]
    return out
```

```python
import os
import numpy as np
import ml_dtypes
from contextlib import ExitStack
import concourse.bass as bass
import concourse.mybir as mybir
from concourse.bass_utils import run_bass_kernel_spmd

F32 = mybir.dt.float32
BF16 = mybir.dt.bfloat16
AF = mybir.ActivationFunctionType
ALU = mybir.AluOpType
AX = mybir.AxisListType
NPBF = ml_dtypes.bfloat16

DM = 4096
SEQ = 4096
OWN = 2048
CTX = 4096
DFF = 11008
DIN = 11312
EPS = 1e-5
SCALE = 128 ** -0.5
NTT = 4
TT = 512


class Sched:
    def __init__(self, nc, stack):
        self.nc = nc
        self.stack = stack
        self.eng = {"pe": nc.tensor, "dve": nc.vector, "act": nc.scalar, "pool": nc.gpsimd, "sp": nc.sync}
        self.sem = {}
        self.cnt = {}
        self.known = {e: {} for e in self.eng}
        self.lastw = {}
        self.readers = {}
        self.nwaits = 0

    def _key(self, key):
        if key not in self.sem:
            self.sem[key] = self.stack.enter_context(self.nc.semaphore("s_" + key))
            self.cnt[key] = 0
        return key

    def _collect(self, e, reads, writes):
        deps = {}

        def need(k, v, raw):
            if k == e and not raw:
                return
            if deps.get(k, 0) < v:
                deps[k] = v

        for r in reads:
            w = self.lastw.get(r)
            if w:
                need(w[0], w[1], True)
        for r in writes:
            w = self.lastw.get(r)
            if w:
                need(w[0], w[1], False)
            for k, v in self.readers.get(r, {}).items():
                need(k, v, False)
        for k, v in deps.items():
            if self.known[e].get(k, 0) < v:
                self.eng[e].wait_ge(self.sem[k], v)
                self.known[e][k] = v
                self.nwaits += 1

    def _record(self, key, reads, writes):
        v = self.cnt[key]
        for r in reads:
            self.readers.setdefault(r, {})[key] = v
        for w in writes:
            self.lastw[w] = (key, v)
            self.readers[w] = {}

    def op(self, e, fn, reads=(), writes=()):
        self._key(e)
        self._collect(e, reads, writes)
        ins = fn()
        self.cnt[e] += 1
        ins.then_inc(self.sem[e], 1)
        self._record(e, reads, writes)

    def dma(self, q, key, out, in_, reads=(), writes=()):
        key = self._key("d_" + key)
        self._collect(q, reads, writes)
        ins = self.eng[q].dma_start(out=out, in_=in_)
        self.cnt[key] += 16
        ins.then_inc(self.sem[key], 16)
        self._record(key, reads, writes)

    def barrier(self, engines=("pe", "dve", "act", "pool", "sp")):
        for e in engines:
            for k, v in self.cnt.items():
                if k == e or k.startswith("d_cast"):
                    continue
                if self.known[e].get(k, 0) < v:
                    self.eng[e].wait_ge(self.sem[k], v)
                    self.known[e][k] = v

    def final_wait(self, e="sp"):
        for k, v in self.cnt.items():
            if k.startswith("d_") and self.known[e].get(k, 0) < v:
                self.eng[e].wait_ge(self.sem[k], v)
                self.known[e][k] = v


def dap(t, offset, pattern):
    return bass.AP(t, offset, [list(p) for p in pattern])


C_QA, C_KA, C_VA, C_QB = 0, 2048, 4096, 6144
C_KBC, C_VBC, C_KBS, C_VBS, C_KBW, C_VBW, C_GL = 8192, 8704, 9216, 9728, 10240, 10752, 11264
KT_KA, KT_KBC, KT_KBS, KT_KBW, KT_VBC = 0, 16, 20, 24, 28
V_VA, V_VBS, V_VBW = 0, 2048, 2560


def build_program(debug=False, stop_after=None, tiles=range(8), tiles_d=range(4), tiles_c=range(4), skip_attn=False):
    nc = bass.Bass("TRN2", target_bir_lowering=False)
    stack = ExitStack()
    S = Sched(nc, stack)
    dk = "ExternalOutput" if debug else "Internal"

    def din(name, shape, dt=F32):
        return nc.dram_tensor(name, list(shape), dt, kind="ExternalInput")

    xc = din("xc", [CTX, DM])
    w_in = din("w_in", [DM, DIN])
    w_out = din("w_out", [DM, DM])
    w_gate = din("w_gate", [DM, DFF])
    w_up = din("w_up", [DM, DFF])
    w_down = din("w_down", [DFF, DM])
    cw1 = [din("ck_w1", [4096, 128]), din("cv_w1", [4096, 128])]
    cw2 = [din("ck_w2", [128, 128]), din("cv_w2", [128, 128])]
    cpeT = [din("ck_peT", [128, 32]), din("cv_peT", [128, 32])]
    g_attn = din("g_attn", [1, DM])
    g_ffn = din("g_ffn", [1, DM])
    g_fin = din("g_fin", [1, DM])
    g_outT = din("g_outT", [128, 32])
    ropeC = din("ropeC", [128, CTX])
    ropeS = din("ropeS", [128, CTX])
    mA_d = din("mA", [128, 3072], BF16)
    mW_d = din("mW", [128, 1408], BF16)
    mC_d = din("mC", [128, 1024], BF16)
    cmask_d = din("cmaskT", [128, 2, OWN], BF16)
    span_d = din("span", [128, 2, 64], BF16)
    VM_d = din("VM", [128, 16, 64])
    FB_d = din("FB", [128, 16, 64])
    eexp_d = din("eexp", [128, 32, 128], BF16)
    kval_d = din("kval", [128, 2, 128], BF16)
    ident_d = din("ident", [128, 128], BF16)
    rm_d = din("rm", [128, 128], BF16)
    y = nc.dram_tensor("y", [OWN, DM], F32, kind="ExternalOutput")
    qT = nc.dram_tensor("qT", [32, 128, OWN], BF16, kind=dk)
    kT = nc.dram_tensor("kT", [32, 128, CTX], BF16, kind=dk)
    vv = nc.dram_tensor("vv", [CTX, 3072], BF16, kind=dk)
    gTd = nc.dram_tensor("gTd", [48, OWN], F32, kind=dk)
    kcT_d = nc.dram_tensor("kcT", [4, 128, 256], BF16, kind=dk)
    vc_d = nc.dram_tensor("vc", [4, 256, 128], BF16, kind=dk)
    oT = nc.dram_tensor("oT", [32, 128, OWN], BF16, kind=dk)
    x1d = nc.dram_tensor("x1d", [OWN, DM], F32, kind=dk)
    wb_in = nc.dram_tensor("wb_in", [23, 128, 32 * 512], BF16, kind="Internal")
    wb_o = nc.dram_tensor("wb_o", [16, 128, 32 * 256], BF16, kind="Internal")
    wb_g = nc.dram_tensor("wb_g", [43, 128, 32 * 256], BF16, kind="Internal")
    wb_u = nc.dram_tensor("wb_u", [43, 128, 32 * 256], BF16, kind="Internal")
    wb_d = nc.dram_tensor("wb_d", [8, 22, 128, 4 * 512], BF16, kind="Internal")
    ck = [0]

    def mk_cast(dst3, src3, res, nchunk, dimlen):
        def job():
            key = "cast%d" % (ck[0] % 3)
            ck[0] += 1
            dk_ = "d_" + key
            if dk_ in S.cnt and S.known["pool"].get(dk_, 0) < S.cnt[dk_]:
                S.eng["pool"].wait_ge(S.sem[dk_], S.cnt[dk_])
                S.known["pool"][dk_] = S.cnt[dk_]
            step = dimlen // nchunk
            for k in range(nchunk):
                S.dma("pool", key, dst3[:, k * step:(k + 1) * step, :], src3[:, k * step:(k + 1) * step, :], writes=[res])
        return job

    cast_q = []
    for cb in range(16):
        cast_q.append(mk_cast(wb_o.ap()[cb].rearrange("p (h c) -> p h c", h=32),
                              w_out.ap()[:, cb * 256:(cb + 1) * 256].rearrange("(h p) c -> p h c", p=128), "wbo%d" % cb, 4, 32))
    for fb in range(43):
        cast_q.append(mk_cast(wb_g.ap()[fb].rearrange("p (h c) -> p h c", h=32),
                              w_gate.ap()[:, fb * 256:(fb + 1) * 256].rearrange("(h p) c -> p h c", p=128), "wbg%d" % fb, 4, 32))
    cast_d = []
    for fb in range(43):
        cast_d.append(mk_cast(wb_u.ap()[fb].rearrange("p (h c) -> p h c", h=32),
                              w_up.ap()[:, fb * 256:(fb + 1) * 256].rearrange("(h p) c -> p h c", p=128), "wbu%d" % fb, 4, 32))
    for cb in range(8):
        for f4 in range(22):
            nf = 4 if f4 < 21 else 2
            r0 = f4 * 4 * 128
            cast_d.append(mk_cast(wb_d.ap()[cb, f4].rearrange("p (f c) -> p f c", f=4)[:, 0:nf, :],
                                  w_down.ap()[r0:r0 + nf * 128, cb * 512:(cb + 1) * 512].rearrange("(f p) c -> p f c", p=128),
                                  "wbd%d_%d" % (cb, f4), 1, nf))

    uid = [0]

    def sb(name, shape, dt, st=None):
        uid[0] += 1
        return (st or stack).enter_context(nc.sbuf_tensor("sb%d_%s" % (uid[0], name), list(shape), dt))

    def ps(name, shape, dt, st=None):
        uid[0] += 1
        return (st or stack).enter_context(nc.psum_tensor("ps%d_%s" % (uid[0], name), list(shape), dt))

    ident = sb("ident", [128, 128], BF16)
    rm = sb("rm", [128, 128], BF16)
    ones = sb("ones", [128, 128], BF16)
    S.dma("sp", "c0", ident[:], ident_d.ap(), writes=["ident"])
    S.dma("sp", "c1", rm[:], rm_d.ap(), writes=["rm"])
    S.op("dve", lambda: nc.vector.memset(ones[:], 1.0), writes=["ones"])
    epsb = sb("epsb", [128, 1], F32)
    S.op("dve", lambda: nc.vector.memset(epsb[:], EPS), writes=["epsb"])

    def norm_transpose(st_name, xin, gain_bc, xs, hT, blk, tp, ssq, rstd, tpi):
        S.op("act", lambda: nc.scalar.activation(out=xs[:], in_=xin, func=AF.Square, scale=1.0 / 64.0, accum_out=ssq[:]),
             reads=[st_name], writes=["xs", "ssq"])
        S.op("act", lambda: nc.scalar.activation(out=ssq[:], in_=ssq[:], func=AF.Sqrt, bias=epsb[:]),
             reads=["ssq", "epsb"], writes=["ssq"])
        S.op("dve", lambda: nc.vector.reciprocal(out=rstd[:], in_=ssq[:]), reads=["ssq"], writes=["rstd"])
        S.op("dve", lambda: nc.vector.scalar_tensor_tensor(out=xs[:], in0=xin, scalar=rstd[:, 0:1], in1=gain_bc[:],
                                                           op0=ALU.mult, op1=ALU.mult),
             reads=[st_name, "rstd", "gain"], writes=["xs"])
        for g8 in range(4):
            t = tp[tpi[0] % 2]
            tn = "tp%d" % (tpi[0] % 2)
            tpi[0] += 1
            for j in range(8):
                kc = g8 * 8 + j
                S.op("pe", lambda kc=kc, j=j, t=t: nc.tensor.transpose(out=t[:, j * 128:(j + 1) * 128],
                                                                     in_=xs[:, kc * 128:(kc + 1) * 128],
                                                                     identity=ident[:]),
                     reads=["xs", "ident"], writes=[tn])
            dst = hT[:, g8 * 8:(g8 + 1) * 8, blk * 128:(blk + 1) * 128]
            src = t[:].rearrange("p (a b) -> p a b", a=8)
            if g8 % 2 == 0:
                S.op("act", lambda dst=dst, src=src: nc.scalar.copy(out=dst, in_=src), reads=[tn], writes=["hT"])
            else:
                S.op("dve", lambda dst=dst, src=src: nc.vector.tensor_copy(out=dst, in_=src), reads=[tn], writes=["hT"])

    with ExitStack() as pa:
        gain = sb("gainA", [128, DM], F32, pa)
        xblk = [sb("xblk%d" % i, [128, DM], F32, pa) for i in range(2)]
        xs = sb("xsA", [128, DM], BF16, pa)
        hT = sb("hTA", [128, 32, TT], BF16, pa)
        Wt = [sb("WtA%d" % i, [128, 32, 512], BF16, pa) for i in range(2)]
        rC = sb("rC", [128, TT], F32, pa)
        rS = sb("rS", [128, TT], F32, pa)
        qsb = [sb("qsb%d" % i, [128, TT], BF16, pa) for i in range(4)]
        t1 = [sb("t1_%d" % i, [128, TT], F32, pa) for i in range(2)]
        t2 = [sb("t2_%d" % i, [128, TT], F32, pa) for i in range(2)]
        gsb = sb("gsb", [128, TT], F32, pa)
        ssq = sb("ssqA", [128, 1], F32, pa)
        rstd = sb("rstdA", [128, 1], F32, pa)
        tp = [ps("tpA%d" % i, [128, 1024], BF16, pa) for i in range(2)]
        acc = [ps("accA%d" % i, [128, 512], F32, pa) for i in range(4)]
        ps2 = ps("ps2A", [128, 512], F32, pa)

        S.dma("sp", "gain", gain[:], dap(g_attn, 0, [[0, 128], [1, DM]]), writes=["gain"])
        tpi = [0]
        wi = [0]
        acci = [0]
        qi = [0]
        ti = [0]
        blocks = []
        for j in range(4):
            blocks.append((C_QA + 512 * j, 512, "q", 4 * j, True, True))
        for j in range(4):
            blocks.append((C_KA + 512 * j, 512, "k", KT_KA + 4 * j, True, False))
        for j in range(4):
            blocks.append((C_VA + 512 * j, 512, "v", V_VA + 512 * j, False, False))
        for j in range(4):
            blocks.append((C_QB + 512 * j, 512, "q", 16 + 4 * j, True, True))
        blocks.append((C_KBC, 512, "k", KT_KBC, True, False))
        blocks.append((C_VBC, 512, "k", KT_VBC, False, False))
        blocks.append((C_KBS, 512, "k", KT_KBS, True, False))
        blocks.append((C_VBS, 512, "v", V_VBS, False, False))
        blocks.append((C_KBW, 512, "k", KT_KBW, True, False))
        blocks.append((C_VBW, 512, "v", V_VBW, False, False))
        blocks.append((C_GL, 48, "g", 0, False, True))

        cast_done = set()
        for tile in tiles:
            own = tile >= 4
            tok0 = tile * TT
            for blk in range(4):
                xb = xblk[(tile * 4 + blk) % 2]
                xn = "xblk%d" % ((tile * 4 + blk) % 2)
                r0 = tok0 + blk * 128
                S.dma("sp", xn, xb[:], xc.ap()[r0:r0 + 128, :], writes=[xn])
                norm_transpose(xn, xb[:], gain, xs, hT, blk, tp, ssq, rstd, tpi)
            S.dma("sp", "rC", rC[:], ropeC.ap()[:, tok0:tok0 + TT], writes=["rC"])
            S.dma("sp", "rS", rS[:], ropeS.ap()[:, tok0:tok0 + TT], writes=["rS"])

            def post_fm(bank, bn, kind, dest, rope, hh):
                if os.environ.get("KNOPOST"):
                    return
                if os.environ.get("KNOROPE"):
                    rope = False
                s = qi[0] % 4
                qi[0] += 1
                q = qsb[s]
                qn = "qsb%d" % s
                S.op("act", lambda: nc.scalar.copy(out=q[:], in_=bank[:]), reads=[bn], writes=[qn])
                if rope:
                    u = ti[0] % 2
                    ti[0] += 1
                    S.op("pe", lambda: nc.tensor.matmul(ps2[:], lhsT=rm[:], rhs=q[:], start=True, stop=True),
                         reads=[qn, "rm"], writes=["ps2"])
                    S.op("dve", lambda: nc.vector.tensor_tensor(out=t1[u][:], in0=bank[:], in1=rC[:], op=ALU.mult),
                         reads=[bn, qn, "rC"], writes=["t1_%d" % u])
                    S.op("dve", lambda: nc.vector.tensor_tensor(out=t2[u][:], in0=ps2[:], in1=rS[:], op=ALU.mult),
                         reads=["ps2", "rS"], writes=["t2_%d" % u])
                    S.op("dve", lambda: nc.vector.tensor_tensor(out=q[:], in0=t1[u][:], in1=t2[u][:], op=ALU.add),
                         reads=["t1_%d" % u, "t2_%d" % u], writes=[qn])
                if kind == "q":
                    dst = qT.ap()[dest + hh, :, tok0 - OWN:tok0 - OWN + TT]
                else:
                    dst = kT.ap()[dest + hh, :, tok0:tok0 + TT]
                S.dma("sp", qn, dst, q[:], reads=[qn], writes=["dram_qk"])

            def post_v(bank, bn, vcol, tb):
                s = qi[0] % 4
                qi[0] += 1
                q = qsb[s]
                qn = "qsb%d" % s
                S.op("act", lambda: nc.scalar.copy(out=q[:], in_=bank[:]), reads=[bn], writes=[qn])
                r0 = tok0 + tb * 128
                S.dma("sp", qn, vv.ap()[r0:r0 + 128, vcol:vcol + 512], q[:], reads=[qn], writes=["dram_v"])

            def post_g(bank, bn):
                S.op("act", lambda: nc.scalar.activation(out=gsb[:], in_=bank[:], func=AF.Sigmoid),
                     reads=[bn], writes=["gsb"])
                S.dma("sp", "gsb", gTd.ap()[:, tok0 - OWN:tok0 - OWN + TT], gsb[0:48, :], reads=["gsb"], writes=["dram_g"])

            pending = None
            KD = int(os.environ.get("KDBG", "0"))
            for bi, (c0, ncol, kind, dest, rope, own_only) in enumerate(blocks):
                if own_only and not own:
                    continue
                if KD == 1 or (KD >= 2 and bi not in (0, 8, 22)[:KD - 1]):
                    continue
                w = Wt[wi[0] % 2]
                wn = "WtA%d" % (wi[0] % 2)
                wi[0] += 1
                if kind == "g":
                    gsrc_ = w_in.ap()[:, c0:c0 + ncol].rearrange("(kc p) c -> p kc c", p=128)
                    for k4 in range(8):
                        S.dma("pool", wn, w[:, 4 * k4:4 * k4 + 4, 0:ncol], gsrc_[:, 4 * k4:4 * k4 + 4, :], writes=[wn])
                elif bi not in cast_done:
                    cast_done.add(bi)
                    mk_cast(wb_in.ap()[bi].rearrange("p (kc c) -> p kc c", kc=32)[:, :, 0:ncol],
                            w_in.ap()[:, c0:c0 + ncol].rearrange("(kc p) c -> p kc c", p=128), "wbin%d" % bi, 8, 32)()
                elif cast_q:
                    cast_q.pop(0)()
                wsrc = wb_in.ap()[bi].rearrange("p (kc c) -> p kc c", kc=32)
                for k4 in range(4 if kind != "g" else 0):
                    S.dma("pool", wn, w[:, 8 * k4:8 * k4 + 8, 0:ncol], wsrc[:, 8 * k4:8 * k4 + 8, 0:ncol], reads=["wbin%d" % bi], writes=[wn])
                nunits = 1 if kind == "g" else 4
                for un in range(nunits):
                    b = acci[0] % 4
                    acci[0] += 1
                    bank = acc[b]
                    bn = "accA%d" % b
                    for kc in range(32):
                        if kind in ("q", "k"):
                            S.op("pe", lambda kc=kc, un=un: nc.tensor.matmul(bank[:], lhsT=w[:, kc, un * 128:(un + 1) * 128],
                                                                        rhs=hT[:, kc, :], start=(kc == 0), stop=(kc == 31)),
                                 reads=[wn, "hT"], writes=[bn])
                        elif kind == "v":
                            S.op("pe", lambda kc=kc, un=un: nc.tensor.matmul(bank[:], lhsT=hT[:, kc, un * 128:(un + 1) * 128],
                                                                        rhs=w[:, kc, :], start=(kc == 0), stop=(kc == 31)),
                                 reads=[wn, "hT"], writes=[bn])
                        else:
                            S.op("pe", lambda kc=kc: nc.tensor.matmul(bank[:], lhsT=w[:, kc, 0:128],
                                                                 rhs=hT[:, kc, :], start=(kc == 0), stop=(kc == 31)),
                                 reads=[wn, "hT"], writes=[bn])
                    if pending is not None:
                        pending()
                    if kind in ("q", "k"):
                        pending = (lambda bank=bank, bn=bn, kind=kind, dest=dest, rope=rope, un=un:
                                   post_fm(bank, bn, kind, dest, rope, un))
                    elif kind == "v":
                        pending = (lambda bank=bank, bn=bn, dest=dest, un=un: post_v(bank, bn, dest, un))
                    else:
                        pending = (lambda bank=bank, bn=bn: post_g(bank, bn))
            if pending is not None:
                pending()
            if tile == list(tiles)[0]:
                pre = []
                for bi2, (c02, ncol2, _k, _d, _r, _o) in enumerate(blocks):
                    if bi2 not in cast_done and _k != "g":
                        cast_done.add(bi2)
                        pre.append(mk_cast(wb_in.ap()[bi2].rearrange("p (kc c) -> p kc c", kc=32)[:, :, 0:ncol2],
                                           w_in.ap()[:, c02:c02 + ncol2].rearrange("(kc p) c -> p kc c", p=128), "wbin%d" % bi2, 8, 32))
                cast_q[0:0] = pre
        while cast_q:
            cast_q.pop(0)()
        for j in cast_d:
            j()
        S.barrier()
    if stop_after == "A":
        return finish(nc, S, stack, y)


    if not skip_attn:
        attention_phases(nc, S, stack, sb, ps, dict(
            qT=qT, kT=kT, vv=vv, gTd=gTd, oT=oT, cw1=cw1, cw2=cw2, cpeT=cpeT, mA=mA_d, mW=mW_d, mC=mC_d,
            cmask=cmask_d, span=span_d, VM=VM_d, FB=FB_d, eexp=eexp_d, kval=kval_d, ones=ones, ident=ident),
            tiles_c)
    else:
        with ExitStack() as pz:
            zt = sb("zeroT", [128, OWN], BF16, pz)
            S.op("dve", lambda: nc.vector.memset(zt[:], 0.0), writes=["zt"])
            for h in range(32):
                S.dma("sp", "zt", oT.ap()[h, :, :], zt[:], reads=["zt"], writes=["dram_oT"])
            S.barrier()
    gout = sb("gout", [128, 32], F32)
    S.dma("sp", "c2", gout[:], g_outT.ap(), writes=["gout"])
    for t in tiles_d:
        q0 = t * TT
        with ExitStack() as pt:
          h2T = sb("h2T", [128, 32, TT], BF16, pt)
          with ExitStack() as pdx:
           x1 = sb("x1D", [128, 4, DM], F32, pdx)
           with ExitStack() as pd:
            OT = sb("OT", [128, 32, TT], BF16, pd)
            sq = [sb("sqD%d" % i, [128, 8, TT], BF16, pd) for i in range(2)]
            rs = [sb("rsD%d" % i, [128, TT], F32, pd) for i in range(2)]
            Wo = [sb("WoD%d" % i, [128, 32, 256], BF16, pd) for i in range(2)]
            xr = [sb("xrD%d" % i, [128, 4, 256], F32, pd) for i in range(2)]
            ssp = [ps("sspD%d" % i, [128, TT], F32, pd) for i in range(2)]
            acc = [ps("accD%d" % i, [128, 256], F32, pd) for i in range(4)]
            S.dma("sp", "OT", OT[:], oT.ap()[:, :, q0:q0 + TT].rearrange("h d t -> d h t"), reads=["dram_oT"], writes=["OT"])
            for g in range(2):
                for i8 in range(2):
                    u = (g * 2 + i8) % 2
                    h0 = g * 16 + i8 * 8
                    S.op("act", lambda u=u, h0=h0: nc.scalar.activation(out=sq[u][:], in_=OT[:, h0:h0 + 8, :], func=AF.Square),
                         reads=["OT"], writes=["sq%d" % u])
                    for j in range(8):
                        S.op("pe", lambda u=u, j=j, g=g, i8=i8: nc.tensor.matmul(ssp[g][:], lhsT=ones[:], rhs=sq[u][:, j, :],
                                                                          start=(i8 == 0 and j == 0), stop=(i8 == 1 and j == 7)),
                             reads=["sq%d" % u, "ones"], writes=["ssp%d" % g])
                S.op("act", lambda g=g: nc.scalar.activation(out=rs[g][:], in_=ssp[g][:], func=AF.Sqrt, scale=1.0 / 2048.0, bias=epsb[:]),
                     reads=["ssp%d" % g, "epsb"], writes=["rs%d" % g])
                S.op("dve", lambda g=g: nc.vector.reciprocal(out=rs[g][:], in_=rs[g][:]), reads=["rs%d" % g], writes=["rs%d" % g])
            for h in range(32):
                S.op("dve", lambda h=h: nc.vector.scalar_tensor_tensor(out=OT[:, h, :], in0=OT[:, h, :], scalar=gout[:, h:h + 1],
                                                                  in1=rs[h // 16][:], op0=ALU.mult, op1=ALU.mult),
                     reads=["OT", "gout", "rs%d" % (h // 16)], writes=["OT"])
            for cb in range(16):
                w = Wo[cb % 2]
                wn = "WoD%d" % (cb % 2)
                c0 = cb * 256
                wsrc = wb_o.ap()[cb].rearrange("p (h c) -> p h c", h=32)
                for k4 in range(2):
                    S.dma("sp", wn, w[:, 16 * k4:16 * k4 + 16, :], wsrc[:, 16 * k4:16 * k4 + 16, :], reads=["wbo%d" % cb], writes=[wn])
                xrt = xr[cb % 2]
                xn = "xrD%d" % (cb % 2)
                S.dma("sp", xn, xrt[:], xc.ap()[OWN + q0:OWN + q0 + TT, c0:c0 + 256].rearrange("(b p) c -> p b c", p=128),
                      writes=[xn])
                for tb in range(4):
                    for h in range(32):
                        S.op("pe", lambda tb=tb, h=h: nc.tensor.matmul(acc[tb][:], lhsT=OT[:, h, tb * 128:(tb + 1) * 128], rhs=w[:, h, :],
                                                                  start=(h == 0), stop=(h == 31)),
                             reads=["OT", wn], writes=["accD%d" % tb])
                    S.op("dve", lambda tb=tb: nc.vector.tensor_tensor(out=x1[:, tb, c0:c0 + 256], in0=acc[tb][:], in1=xrt[:, tb, :], op=ALU.add),
                         reads=["accD%d" % tb, xn], writes=["x1"])
            S.dma("sp", "x1o", x1d.ap()[q0:q0 + TT, :].rearrange("(b p) c -> p b c", p=128), x1[:], reads=["x1"], writes=["dram_x1"])
            S.barrier()
           if True:
            with ExitStack() as pd3:
                gainF = sb("gainF", [128, DM], F32, pd3)
                xs = sb("xsD", [128, DM], BF16, pd3)
                ssq = sb("ssqD", [128, 1], F32, pd3)
                rstd = sb("rstdD", [128, 1], F32, pd3)
                tp = [ps("tpD%d" % i, [128, 1024], BF16, pd3) for i in range(2)]
                S.dma("sp", "gain", gainF[:], dap(g_ffn, 0, [[0, 128], [1, DM]]), writes=["gain"])
                tpi = [0]
                for tb in range(4):
                    norm_transpose("x1", x1[:, tb, :], gainF, xs, h2T, tb, tp, ssq, rstd, tpi)
                S.barrier()
          ffn_tile(nc, S, stack, sb, ps, t, q0, h2T, wb_g, wb_u, wb_d, x1d, g_fin, y, epsb)
          S.barrier()
    return finish(nc, S, stack, y)


def attention_phases(nc, S, stack, sb, ps, T, tiles_c):
    qT, kT, vv, gTd, oT = T["qT"], T["kT"], T["vv"], T["gTd"], T["oT"]
    ones, ident = T["ones"], T["ident"]
    EXP = AF.Exp
    kval = sb("kval", [128, 2, 128], BF16)
    S.dma("sp", "c3", kval[:], T["kval"].ap(), writes=["kval"])
    kcT_all = sb("kcT_all", [128, 4, 256], BF16)
    vc_all = sb("vc_all", [128, 4, 2, 128], BF16)

    with ExitStack() as pb:
        XT = sb("XTB", [128, CTX + 32], BF16, pb)
        W1 = sb("W1B", [128, 32, 128], BF16, pb)
        W2 = sb("W2B", [128, 128], BF16, pb)
        W1f = sb("W1f", [128, 32, 128], F32, pb)
        W2f = sb("W2f", [128, 128], F32, pb)
        pe32 = sb("pe32", [128, 32], F32, pb)
        pe16 = sb("pe16", [128, 32], BF16, pb)
        b1 = sb("b1B", [128, 1], F32, pb)
        xg = sb("xgB", [128, 256], F32, pb)
        x2 = sb("x2B", [128, 256], F32, pb)
        gTb = sb("gTB", [128, 256], BF16, pb)
        pb1 = ps("pb1", [128, 512], F32, pb)
        po1 = ps("po1", [128, 512], F32, pb)
        po2 = ps("po2", [128, 512], F32, pb)
        S.op("dve", lambda: nc.vector.memset(XT[:, CTX:CTX + 32], 0.0), writes=["XT"])
        for kind in range(2):
            S.dma("sp", "W1B", W1f[:], T["cw1"][kind].ap().rearrange("(i d) h -> d i h", d=128), writes=["W1f"])
            S.dma("sp", "W2B", W2f[:], T["cw2"][kind].ap(), writes=["W2f"])
            S.op("dve", lambda: nc.vector.tensor_copy(out=W1[:], in_=W1f[:]), reads=["W1f"], writes=["W1"])
            S.op("dve", lambda: nc.vector.tensor_copy(out=W2[:], in_=W2f[:]), reads=["W2f"], writes=["W2"])
            S.dma("sp", "pe32", pe32[:], T["cpeT"][kind].ap(), writes=["pe32"])
            S.op("dve", lambda: nc.vector.tensor_copy(out=pe16[:], in_=pe32[:]), reads=["pe32"], writes=["pe16"])
            for i in range(32):
                S.op("pe", lambda i=i: nc.tensor.matmul(pb1[:, 0:1], lhsT=W1[:, i, :], rhs=pe16[:, i:i + 1], start=(i == 0), stop=(i == 31)),
                     reads=["W1", "pe16"], writes=["pb1"])
            S.op("dve", lambda: nc.vector.tensor_copy(out=b1[:], in_=pb1[:, 0:1]), reads=["pb1"], writes=["b1"])
            for g in range(4):
                slot = (KT_KBC if kind == 0 else KT_VBC) + g
                S.dma("sp", "XTB", XT[:, 0:CTX], kT.ap()[slot, :, :], reads=["dram_qk"], writes=["XT"])
                for i in range(32):
                    rhs = dap(XT, i, [[CTX + 32, 128], [16, 256]])
                    S.op("pe", lambda i=i, rhs=rhs: nc.tensor.matmul(po1[:, 0:256], lhsT=W1[:, i, :], rhs=rhs, start=(i == 0), stop=(i == 31)),
                         reads=["W1", "XT"], writes=["po1"])
                S.op("dve", lambda: nc.vector.tensor_scalar(out=xg[:], in0=po1[:, 0:256], scalar1=b1[:, 0:1], scalar2=None, op0=ALU.add),
                     reads=["po1", "b1"], writes=["xg"])
                S.op("act", lambda: nc.scalar.activation(out=x2[:], in_=xg[:], func=AF.Square), reads=["xg"], writes=["x2"])
                S.op("dve", lambda: nc.vector.tensor_scalar(out=x2[:], in0=x2[:], scalar1=0.044715, scalar2=1.0, op0=ALU.mult, op1=ALU.add),
                     reads=["x2"], writes=["x2"])
                S.op("dve", lambda: nc.vector.tensor_tensor(out=x2[:], in0=x2[:], in1=xg[:], op=ALU.mult), reads=["x2", "xg"], writes=["x2"])
                S.op("act", lambda: nc.scalar.activation(out=x2[:], in_=x2[:], func=AF.Sigmoid, scale=1.5957691216057308),
                     reads=["x2"], writes=["x2"])
                S.op("dve", lambda: nc.vector.tensor_tensor(out=gTb[:], in0=x2[:], in1=xg[:], op=ALU.mult), reads=["x2", "xg"], writes=["gTb"])
                if kind == 0:
                    S.op("pe", lambda: nc.tensor.matmul(po2[:, 0:256], lhsT=W2[:], rhs=gTb[:], start=True, stop=True),
                         reads=["W2", "gTb"], writes=["po2"])
                    S.op("act", lambda g=g: nc.scalar.copy(out=kcT_all[:, g, :], in_=po2[:, 0:256]), reads=["po2"], writes=["kcT_all"])
                else:
                    for cb in range(2):
                        S.op("pe", lambda cb=cb: nc.tensor.matmul(po2[:, cb * 128:(cb + 1) * 128], lhsT=gTb[:, cb * 128:(cb + 1) * 128], rhs=W2[:],
                                                             start=True, stop=True), reads=["W2", "gTb"], writes=["po2"])
                    S.op("act", lambda g=g: nc.scalar.copy(out=vc_all[:, g, :, :], in_=po2[:, 0:256].rearrange("p (a b) -> p a b", a=2)),
                         reads=["po2"], writes=["vc_all"])
        S.barrier()

    def pair(ST, stn, lhsK, rhsQ, E, en, PT, pn, mask_ap, mreads, O, on, lhsV, vreads, Dn, dn, lhsD, dreads, first, last):
        S.op("pe", lambda: nc.tensor.matmul(ST[:], lhsT=lhsK, rhs=rhsQ, start=True, stop=True), reads=vreads[:1] + ["QT"], writes=[stn])
        S.op("act", lambda: nc.scalar.activation(out=E[:], in_=ST[:], func=EXP, scale=SCALE), reads=[stn], writes=[en])
        S.op("dve", lambda: nc.vector.tensor_tensor(out=PT[:], in0=E[:], in1=mask_ap, op=ALU.mult), reads=[en] + mreads, writes=[pn])
        S.op("pe", lambda: nc.tensor.matmul(O[:], lhsT=lhsV, rhs=PT[:], start=first, stop=last), reads=[pn] + vreads[1:], writes=[on])
        S.op("pe", lambda: nc.tensor.matmul(Dn[:], lhsT=lhsD, rhs=PT[:], start=first, stop=last), reads=[pn] + dreads, writes=[dn])

    with ExitStack() as pc:
        mA = sb("mA", [128, 3072], BF16, pc)
        S.dma("sp", "c4", mA[:], T["mA"].ap(), writes=["mA"])
        KT = [sb("KTA%d" % i, [128, CTX], BF16, pc) for i in range(2)]
        VH = [sb("VHA%d" % i, [128, 32, 128], BF16, pc) for i in range(2)]
        QT = [sb("QTA%d" % i, [128, OWN], BF16, pc) for i in range(2)]
        E = [sb("EA%d" % i, [128, TT], BF16, pc) for i in range(3)]
        PT = [sb("PTA%d" % i, [128, TT], BF16, pc) for i in range(3)]
        rden = [sb("rdA%d" % i, [128, TT], F32, pc) for i in range(2)]
        ob = [sb("obA%d" % i, [128, TT], BF16, pc) for i in range(2)]
        ST = [ps("STA%d" % i, [128, TT], F32, pc) for i in range(3)]
        O = [ps("OA%d" % i, [128, TT], F32, pc) for i in range(2)]
        Dn = [ps("DA%d" % i, [128, TT], F32, pc) for i in range(2)]
        pi = 0
        oi = 0
        later = []
        for h in range(16):
            u = h % 2
            S.dma("sp", "KTA%d" % u, KT[u][:], kT.ap()[KT_KA + h, :, :], reads=["dram_qk"], writes=["KT%d" % u])
            S.dma("sp", "VHA%d" % u, VH[u][:], vv.ap()[:, V_VA + h * 128:V_VA + (h + 1) * 128].rearrange("(kb p) d -> p kb d", p=128),
                  reads=["dram_v"], writes=["VH%d" % u])
            S.dma("sp", "QTA%d" % u, QT[u][:], qT.ap()[h, :, :], reads=["dram_qk"], writes=["QT%d" % u])
            for t in tiles_c:
                o = oi % 2
                oi += 1
                kbs = list(range(4 * t, 4 * t + 20))
                for n, kb in enumerate(kbs):
                    D = 16 + 4 * t - kb
                    s3 = pi % 3
                    pi += 1
                    S.op("pe", lambda kb=kb, s3=s3: nc.tensor.matmul(ST[s3][:], lhsT=KT[u][:, kb * 128:(kb + 1) * 128],
                                                                 rhs=QT[u][:, t * TT:(t + 1) * TT], start=True, stop=True),
                         reads=["KT%d" % u, "QT%d" % u], writes=["STA%d" % s3])
                    S.op("act", lambda s3=s3: nc.scalar.activation(out=E[s3][:], in_=ST[s3][:], func=EXP, scale=SCALE),
                         reads=["STA%d" % s3], writes=["EA%d" % s3])
                    S.op("dve", lambda s3=s3, D=D: nc.vector.tensor_tensor(out=PT[s3][:], in0=E[s3][:], in1=mA[:, 128 * D + 384:128 * D + 896], op=ALU.mult),
                         reads=["EA%d" % s3, "mA"], writes=["PTA%d" % s3])
                    def back(kb=kb, s3=s3, n=n, o=o, u=u):
                        S.op("pe", lambda: nc.tensor.matmul(O[o][:], lhsT=VH[u][:, kb, :], rhs=PT[s3][:], start=(n == 0), stop=(n == 19)),
                             reads=["PTA%d" % s3, "VH%d" % u], writes=["OA%d" % o])
                        S.op("pe", lambda: nc.tensor.matmul(Dn[o][:], lhsT=kval[:, 0 if kb < 16 else 1, :], rhs=PT[s3][:],
                                                           start=(n == 0), stop=(n == 19)),
                             reads=["PTA%d" % s3, "kval"], writes=["DA%d" % o])
                    later.append(back)
                    if len(later) > 2:
                        later.pop(0)()
                while later:
                    later.pop(0)()
                S.op("dve", lambda o=o: nc.vector.reciprocal(out=rden[o][:], in_=Dn[o][:]), reads=["DA%d" % o], writes=["rdA%d" % o])
                S.op("dve", lambda o=o: nc.vector.tensor_tensor(out=ob[o][:], in0=O[o][:], in1=rden[o][:], op=ALU.mult),
                     reads=["OA%d" % o, "rdA%d" % o], writes=["obA%d" % o])
                S.dma("sp", "obA%d" % o, oT.ap()[h, :, t * TT:(t + 1) * TT], ob[o][:], reads=["obA%d" % o], writes=["dram_oT"])
        S.barrier()

    with ExitStack() as pn_:
        st = pn_
        mW = sb("mW", [128, 1408], BF16, st)
        mC = sb("mC", [128, 1024], BF16, st)
        cmask = sb("cmask", [128, 2, OWN], BF16, st)
        span = sb("span", [128, 2, 64], BF16, st)
        VM = sb("VM", [128, 16, 64], F32, st)
        FB = sb("FB", [128, 16, 64], F32, st)
        eexp = sb("eexp", [128, 32, 128], BF16, st)
        for nm, tl in (("mW", mW), ("mC", mC), ("cmask", cmask), ("span", span), ("VM", VM), ("FB", FB), ("eexp", eexp)):
            S.dma("sp", "k_" + nm, tl[:], T[nm].ap(), writes=[nm])
        QB = sb("QB", [128, 4, OWN], BF16, st)
        KS = sb("KS", [128, CTX], BF16, st)
        VS = sb("VS", [128, 32, 128], BF16, st)
        KW = sb("KW", [128, CTX], BF16, st)
        VW = sb("VW", [128, 32, 128], BF16, st)
        EC = sb("EC", [128, 4, 2, TT], BF16, st)
        SM = sb("SM", [128, 32, TT], BF16, st)
        accO = [sb("accO%d" % i, [128, TT], F32, st) for i in range(4)]
        G = [sb("G%d" % i, [128, 3, TT], F32, st) for i in range(4)]
        E = [sb("EB%d" % i, [128, TT], BF16, st) for i in range(3)]
        PT = [sb("PTB%d" % i, [128, TT], BF16, st) for i in range(3)]
        rcb = sb("rcb", [128, TT], F32, st)
        coef = sb("coef", [128, TT], F32, st)
        tmpo = sb("tmpo", [128, TT], F32, st)
        sc = sb("sc", [128, 4, 64], F32, st)
        sc2 = sb("sc2", [128, 64], F32, st)
        m8 = sb("m8", [128, 8], F32, st)
        selp = sb("selp", [128, 4, 128], BF16, st)
        self_ = sb("self", [128, 64], F32, st)
        selT = sb("selT", [128, TT], BF16, st)
        obb = [sb("obB%d" % i, [128, TT], BF16, st) for i in range(2)]
        ST = [ps("STB%d" % i, [128, TT], F32, st) for i in range(2)]
        O = ps("OB", [128, TT], F32, st)
        Dn = ps("DB", [128, TT], F32, st)
        IMP = ps("IMP", [128, TT], F32, st)
        TPS = ps("TPS", [128, 1024], BF16, st)
        MB = [ps("MB%d" % i, [128, TT], F32, st) for i in range(2)]
        S.op("dve", lambda: nc.vector.memset(selp[:], 0.0), writes=["selp"])
        pi = 0
        obi = 0

        def branch(r, t, Ksb, kname, Vsb, vname, kbs, mask_of, mreads, den_of, dreads):
            nonlocal pi
            laterb = []
            for n, kb in enumerate(kbs):
                s2 = pi % 2
                s3 = pi % 3
                pi += 1
                S.op("pe", lambda: nc.tensor.matmul(ST[s2][:], lhsT=Ksb[:, kb * 128:(kb + 1) * 128], rhs=QB[:, r, t * TT:(t + 1) * TT],
                                                   start=True, stop=True), reads=[kname, "QB"], writes=["STB%d" % s2])
                S.op("act", lambda: nc.scalar.activation(out=E[s3][:], in_=ST[s2][:], func=EXP, scale=SCALE),
                     reads=["STB%d" % s2], writes=["EB%d" % s3])
                S.op("dve", lambda: nc.vector.tensor_tensor(out=PT[s3][:], in0=E[s3][:], in1=mask_of(kb), op=ALU.mult),
                     reads=["EB%d" % s3] + mreads, writes=["PTB%d" % s3])
                def back(kb=kb, s3=s3, n=n):
                    S.op("pe", lambda: nc.tensor.matmul(O[:], lhsT=Vsb[:, kb, :], rhs=PT[s3][:], start=(n == 0), stop=(n == len(kbs) - 1)),
                         reads=["PTB%d" % s3, vname], writes=["OB"])
                    S.op("pe", lambda: nc.tensor.matmul(Dn[:], lhsT=den_of(kb), rhs=PT[s3][:], start=(n == 0), stop=(n == len(kbs) - 1)),
                         reads=["PTB%d" % s3] + dreads, writes=["DB"])
                laterb.append(back)
                if len(laterb) > 2:
                    laterb.pop(0)()
            while laterb:
                laterb.pop(0)()

        def combine(r, br, first):
            S.op("dve", lambda: nc.vector.reciprocal(out=rcb[:], in_=Dn[:]), reads=["DB"], writes=["rcb"])
            S.op("dve", lambda: nc.vector.tensor_tensor(out=coef[:], in0=rcb[:], in1=G[r][:, br, :], op=ALU.mult),
                 reads=["rcb", "G%d" % r], writes=["coef"])
            if first:
                S.op("dve", lambda: nc.vector.tensor_tensor(out=accO[r][:], in0=O[:], in1=coef[:], op=ALU.mult),
                     reads=["OB", "coef"], writes=["accO%d" % r])
            else:
                S.op("dve", lambda: nc.vector.tensor_tensor(out=tmpo[:], in0=O[:], in1=coef[:], op=ALU.mult),
                     reads=["OB", "coef"], writes=["tmpo"])
                S.op("dve", lambda: nc.vector.tensor_tensor(out=accO[r][:], in0=accO[r][:], in1=tmpo[:], op=ALU.add),
                     reads=["tmpo", "accO%d" % r], writes=["accO%d" % r])

        for g in range(4):
            for r in range(4):
                S.dma("sp", "QB", QB[:, r, :], qT.ap()[16 + 4 * g + r, :, :], reads=["dram_qk"], writes=["QB"])
            S.dma("sp", "KS", KS[:], kT.ap()[KT_KBS + g, :, :], reads=["dram_qk"], writes=["KS"])
            S.dma("sp", "KW", KW[:], kT.ap()[KT_KBW + g, :, :], reads=["dram_qk"], writes=["KW"])
            S.dma("sp", "VS", VS[:], vv.ap()[:, V_VBS + g * 128:V_VBS + (g + 1) * 128].rearrange("(kb p) d -> p kb d", p=128),
                  reads=["dram_v"], writes=["VS"])
            S.dma("sp", "VW", VW[:], vv.ap()[:, V_VBW + g * 128:V_VBW + (g + 1) * 128].rearrange("(kb p) d -> p kb d", p=128),
                  reads=["dram_v"], writes=["VW"])
            for t in tiles_c:
                for r in range(4):
                    S.dma("sp", "G%d" % r, G[r][:], dap(gTd, ((4 * g + r) * 3) * OWN + t * TT, [[0, 128], [OWN, 3], [1, TT]]),
                          reads=["dram_g"], writes=["G%d" % r])
                    for cb in range(2):
                        s2 = pi % 2
                        s3 = pi % 3
                        pi += 1
                        S.op("pe", lambda cb=cb, s2=s2: nc.tensor.matmul(ST[s2][:], lhsT=kcT_all[:, g, cb * 128:(cb + 1) * 128],
                                                                     rhs=QB[:, r, t * TT:(t + 1) * TT], start=True, stop=True),
                             reads=["kcT_all", "QB"], writes=["STB%d" % s2])
                        S.op("act", lambda s2=s2, s3=s3: nc.scalar.activation(out=E[s3][:], in_=ST[s2][:], func=EXP, scale=SCALE),
                             reads=["STB%d" % s2], writes=["EB%d" % s3])
                        S.op("dve", lambda cb=cb, s3=s3: nc.vector.tensor_tensor(out=EC[:, r, cb, :], in0=E[s3][:], in1=cmask[:, cb, t * TT:(t + 1) * TT],
                                                                            op=ALU.mult), reads=["EB%d" % s3, "cmask"], writes=["EC"])
                        S.op("pe", lambda cb=cb: nc.tensor.matmul(O[:], lhsT=vc_all[:, g, cb, :], rhs=EC[:, r, cb, :], start=(cb == 0), stop=(cb == 1)),
                             reads=["EC", "vc_all"], writes=["OB"])
                        S.op("pe", lambda cb=cb: nc.tensor.matmul(Dn[:], lhsT=ones[:], rhs=EC[:, r, cb, :], start=(cb == 0), stop=(cb == 1)),
                             reads=["EC", "ones"], writes=["DB"])
                    S.op("dve", lambda: nc.vector.tensor_scalar(out=rcb[:], in0=Dn[:], scalar1=1e-30, scalar2=None, op0=ALU.max),
                         reads=["DB"], writes=["rcb"])
                    S.op("dve", lambda: nc.vector.reciprocal(out=rcb[:], in_=rcb[:]), reads=["rcb"], writes=["rcb"])
                    S.op("dve", lambda: nc.vector.tensor_tensor(out=coef[:], in0=rcb[:], in1=G[r][:, 0, :], op=ALU.mult),
                         reads=["rcb", "G%d" % r], writes=["coef"])
                    S.op("dve", lambda: nc.vector.tensor_tensor(out=accO[r][:], in0=O[:], in1=coef[:], op=ALU.mult),
                         reads=["OB", "coef"], writes=["accO%d" % r])
                    for cb in range(2):
                        S.op("dve", lambda cb=cb: nc.vector.tensor_tensor(out=EC[:, r, cb, :], in0=EC[:, r, cb, :], in1=rcb[:], op=ALU.mult),
                             reads=["EC", "rcb"], writes=["EC"])
                for qb in range(4):
                    n = 0
                    for r in range(4):
                        for cb in range(2):
                            S.op("pe", lambda qb=qb, r=r, cb=cb, n=n: nc.tensor.matmul(IMP[:, qb * 64:(qb + 1) * 64], lhsT=EC[:, r, cb, qb * 128:(qb + 1) * 128],
                                                                               rhs=span[:, cb, :], start=(n == 0), stop=(n == 7)),
                                 reads=["EC", "span"], writes=["IMP"])
                            n += 1
                S.op("dve", lambda: nc.vector.tensor_tensor(out=sc[:], in0=IMP[:, 0:256].rearrange("p (a b) -> p a b", a=4),
                                                            in1=VM[:, 4 * t:4 * t + 4, :], op=ALU.mult), reads=["IMP", "VM"], writes=["sc"])
                S.op("dve", lambda: nc.vector.tensor_tensor(out=sc[:], in0=sc[:], in1=FB[:, 4 * t:4 * t + 4, :], op=ALU.add),
                     reads=["sc", "FB"], writes=["sc"])
                for qb in range(4):
                    S.op("dve", lambda qb=qb: nc.vector.max(out=m8[:], in_=sc[:, qb, :]), reads=["sc"], writes=["m8"])
                    S.op("dve", lambda qb=qb: nc.vector.match_replace(out=sc2[:], in_to_replace=m8[:], in_values=sc[:, qb, :], imm_value=-3.0e38),
                         reads=["sc", "m8"], writes=["sc2"])
                    S.op("dve", lambda: nc.vector.max(out=m8[:], in_=sc2[:]), reads=["sc2"], writes=["m8"])
                    S.op("dve", lambda qb=qb: nc.vector.tensor_scalar(out=self_[:], in0=sc[:, qb, :], scalar1=m8[:, 7:8], scalar2=None, op0=ALU.is_ge),
                         reads=["sc", "m8"], writes=["self"])
                    S.op("dve", lambda qb=qb: nc.vector.tensor_tensor(out=selp[:, qb, 0:64], in0=self_[:], in1=VM[:, 4 * t + qb, :], op=ALU.mult),
                         reads=["self", "VM"], writes=["selp"])
                for qb in range(4):
                    S.op("pe", lambda qb=qb: nc.tensor.transpose(out=TPS[:, qb * 128:(qb + 1) * 128], in_=selp[:, qb, :], identity=ident[:]),
                         reads=["selp", "ident"], writes=["TPS"])
                S.op("act", lambda: nc.scalar.copy(out=selT[:], in_=TPS[:, 0:TT]), reads=["TPS"], writes=["selT"])
                nkb = 20 + 4 * t
                for kb in range(nkb):
                    D = min(16 + 4 * t - kb, 1)
                    mb = kb % 2
                    S.op("pe", lambda kb=kb, mb=mb: nc.tensor.matmul(MB[mb][:], lhsT=eexp[:, kb, :], rhs=selT[:], start=True, stop=True),
                         reads=["eexp", "selT"], writes=["MB%d" % mb])
                    S.op("dve", lambda kb=kb, mb=mb, D=D: nc.vector.tensor_tensor(out=SM[:, kb, :], in0=MB[mb][:], in1=mC[:, 128 * D + 384:128 * D + 896],
                                                                             op=ALU.mult), reads=["MB%d" % mb, "mC"], writes=["SM"])
                for r in range(4):
                    branch(r, t, KS, "KS", VS, "VS", list(range(nkb)), lambda kb: SM[:, kb, :], ["SM"], lambda kb: ones[:], ["ones"])
                    combine(r, 1, False)
                    branch(r, t, KW, "KW", VW, "VW", list(range(4 * t + 12, 4 * t + 20)),
                           lambda kb: mW[:, 128 * (16 + 4 * t - kb) + 384:128 * (16 + 4 * t - kb) + 896], ["mW"],
                           lambda kb: kval[:, 0 if kb < 16 else 1, :], ["kval"])
                    combine(r, 2, False)
                    o = obi % 2
                    obi += 1
                    S.op("act", lambda o=o, r=r: nc.scalar.copy(out=obb[o][:], in_=accO[r][:]), reads=["accO%d" % r], writes=["obB%d" % o])
                    S.dma("sp", "obB%d" % o, oT.ap()[16 + 4 * g + r, :, t * TT:(t + 1) * TT], obb[o][:], reads=["obB%d" % o], writes=["dram_oT"])
        S.barrier()


def ffn_tile(nc, S, stack, sb, ps, t, q0, h2T, wb_g, wb_u, wb_d, x1d, g_fin, y, epsb):
    with ExitStack() as p4:
        actT = sb("actT", [128, 86, TT], BF16, p4)
        with ExitStack() as p4a:
            Wg = [sb("WgD%d" % i, [128, 32, 256], BF16, p4a) for i in range(2)]
            Wu = [sb("WuD%d" % i, [128, 32, 256], BF16, p4a) for i in range(2)]
            sl = [sb("slD%d" % i, [128, TT], F32, p4a) for i in range(2)]
            gb = [ps("gbD%d" % i, [128, TT], F32, p4a) for i in range(2)]
            ub = [ps("ubD%d" % i, [128, TT], F32, p4a) for i in range(2)]
            for fb in range(43):
                wg, wu = Wg[fb % 2], Wu[fb % 2]
                gn, un_ = "WgD%d" % (fb % 2), "WuD%d" % (fb % 2)
                f0 = fb * 256
                gsrc = wb_g.ap()[fb].rearrange("p (h c) -> p h c", h=32)
                usrc = wb_u.ap()[fb].rearrange("p (h c) -> p h c", h=32)
                for k4 in range(2):
                    S.dma("sp", gn, wg[:, 16 * k4:16 * k4 + 16, :], gsrc[:, 16 * k4:16 * k4 + 16, :], reads=["wbg%d" % fb], writes=[gn])
                    S.dma("sp", un_, wu[:, 16 * k4:16 * k4 + 16, :], usrc[:, 16 * k4:16 * k4 + 16, :], reads=["wbu%d" % fb], writes=[un_])
                for j in range(2):
                    fc = fb * 2 + j
                    b = fc % 2
                    for kc in range(32):
                        S.op("pe", lambda kc=kc: nc.tensor.matmul(gb[b][:], lhsT=wg[:, kc, j * 128:(j + 1) * 128], rhs=h2T[:, kc, :],
                                                             start=(kc == 0), stop=(kc == 31)), reads=[gn, "hT"], writes=["gb%d" % b])
                    for kc in range(32):
                        S.op("pe", lambda kc=kc: nc.tensor.matmul(ub[b][:], lhsT=wu[:, kc, j * 128:(j + 1) * 128], rhs=h2T[:, kc, :],
                                                             start=(kc == 0), stop=(kc == 31)), reads=[un_, "hT"], writes=["ub%d" % b])
                    S.op("act", lambda: nc.scalar.activation(out=sl[b][:], in_=gb[b][:], func=AF.Silu), reads=["gb%d" % b], writes=["sl%d" % b])
                    S.op("dve", lambda: nc.vector.tensor_tensor(out=actT[:, fc, :], in0=ub[b][:], in1=sl[b][:], op=ALU.mult),
                         reads=["ub%d" % b, "sl%d" % b], writes=["actT"])
            S.barrier()
        with ExitStack() as p5:
            Wd = [sb("WdD%d" % i, [128, 4, 512], BF16, p5) for i in range(2)]
            x2 = sb("x2D", [128, 2, 4096], F32, p5)
            x1r = sb("x1rD", [128, 2, 512], F32, p5)
            gfin = sb("gfinD", [128, 4096], F32, p5)
            junk = sb("junkD", [128, 4096], BF16, p5)
            ssq = sb("ssq5", [128, 1], F32, p5)
            rstd = sb("rstd5", [128, 1], F32, p5)
            acc = [ps("acc5_%d" % i, [128, 512], F32, p5) for i in range(4)]
            S.dma("sp", "gain", gfin[:], dap(g_fin, 0, [[0, 128], [1, 4096]]), writes=["gain"])
            wi = 0
            for hf in range(2):
                r_lo = q0 + hf * 256
                for cb in range(8):
                    c0 = cb * 512
                    S.dma("sp", "x1r", x1r[:], x1d.ap()[r_lo:r_lo + 256, c0:c0 + 512].rearrange("(b p) c -> p b c", p=128),
                          reads=["dram_x1"], writes=["x1r"])
                    for f4 in range(22):
                        nf = 4 if f4 < 21 else 2
                        w = Wd[wi % 2]
                        wn = "WdD%d" % (wi % 2)
                        wi += 1
                        r0 = f4 * 4 * 128
                        wsrc = wb_d.ap()[cb, f4].rearrange("p (f c) -> p f c", f=4)[:, 0:nf, :]
                        S.dma("sp", wn, w[:, 0:nf, :], wsrc, reads=["wbd%d_%d" % (cb, f4)], writes=[wn])
                        for fl in range(nf):
                            fc = f4 * 4 + fl
                            for tb in range(2):
                                bk = (cb % 2) * 2 + tb
                                tg = hf * 2 + tb
                                S.op("pe", lambda fl=fl, fc=fc, tg=tg, bk=bk: nc.tensor.matmul(
                                    acc[bk][:], lhsT=actT[:, fc, tg * 128:(tg + 1) * 128], rhs=w[:, fl, :],
                                    start=(fc == 0), stop=(fc == 85)), reads=["actT", wn], writes=["acc5_%d" % bk])
                    for tb in range(2):
                        bk = (cb % 2) * 2 + tb
                        S.op("dve", lambda tb=tb, bk=bk: nc.vector.tensor_tensor(out=x2[:, tb, c0:c0 + 512], in0=acc[bk][:], in1=x1r[:, tb, :], op=ALU.add),
                             reads=["acc5_%d" % bk, "x1r"], writes=["x2"])
                for tb in range(2):
                    S.op("act", lambda tb=tb: nc.scalar.activation(out=junk[:], in_=x2[:, tb, :], func=AF.Square, scale=1.0 / 64.0, accum_out=ssq[:]),
                         reads=["x2"], writes=["junk", "ssq"])
                    S.op("act", lambda: nc.scalar.activation(out=ssq[:], in_=ssq[:], func=AF.Sqrt, bias=epsb[:]), reads=["ssq", "epsb"], writes=["ssq"])
                    S.op("dve", lambda: nc.vector.reciprocal(out=rstd[:], in_=ssq[:]), reads=["ssq"], writes=["rstd"])
                    S.op("dve", lambda tb=tb: nc.vector.scalar_tensor_tensor(out=x2[:, tb, :], in0=x2[:, tb, :], scalar=rstd[:, 0:1], in1=gfin[:],
                                                                        op0=ALU.mult, op1=ALU.mult), reads=["x2", "rstd", "gain"], writes=["x2"])
                S.dma("sp", "yo", y.ap()[r_lo:r_lo + 256, :].rearrange("(b p) c -> p b c", p=128), x2[:], reads=["x2"], writes=["dram_y"])
            S.barrier()


def finish(nc, S, stack, y):
    S.final_wait("sp")
    stack.close()
    return nc


def _consts(half):
    p = np.arange(128)[:, None]
    c = {}
    u = np.arange(3072)[None, :]
    d = u - 384 - p
    m = ((d >= 0) & (d <= 128)).astype(np.float32) + ((d >= 0) & (d <= 512) & (d % 4 == 0)) + \
        ((d >= 0) & (d <= 2048) & (d % 16 == 0))
    c["mA"] = m.astype(NPBF)
    u = np.arange(1408)[None, :]
    d = u - 384 - p
    c["mW"] = ((d >= 0) & (d <= 511)).astype(NPBF)
    u = np.arange(1024)[None, :]
    d = u - 384 - p
    c["mC"] = (d >= 0).astype(NPBF)
    cp = np.arange(256)
    qctx = OWN + np.arange(OWN)
    cvalid = (cp <= 254) & ((cp >= 128) | (half == 1))
    cm = ((16 * cp[:, None] + 31) <= qctx[None, :]) & cvalid[:, None]
    c["cmaskT"] = np.ascontiguousarray(cm.reshape(2, 128, OWN).transpose(1, 0, 2)).astype(NPBF)
    j = np.arange(64)
    sh = ((16 * cp[:, None]) < (64 * j[None, :] + 64)) & ((16 * cp[:, None] + 32) > 64 * j[None, :])
    c["span"] = np.ascontiguousarray(sh.reshape(2, 128, 64).transpose(1, 0, 2)).astype(NPBF)
    jabs = j[None, :] - (32 if half == 0 else 0)
    qblk_ctx = (qctx // 64)[:, None]
    j0 = 32 if half == 0 else 0
    valid = (j[None, :] * 64 <= qctx[:, None]) & (jabs >= 0)
    forced = ((j[None, :] == j0) | (j[None, :] == qblk_ctx) | (j[None, :] == qblk_ctx - 1)) & valid
    VM = valid.astype(np.float32)
    FB = np.where(forced, 1e6, np.where(valid, 0.0, -1e30)).astype(np.float32)
    c["VM"] = np.ascontiguousarray(VM.reshape(16, 128, 64).transpose(1, 0, 2))
    c["FB"] = np.ascontiguousarray(FB.reshape(16, 128, 64).transpose(1, 0, 2))
    kb = np.arange(32)
    k = np.arange(128)
    ee = (j[:, None, None] == (2 * kb[None, :, None] + (k[None, None, :] // 64)))
    c["eexp"] = np.concatenate([ee, np.zeros_like(ee)], 0).astype(NPBF)
    kv = np.ones((128, 2, 128), np.float32)
    kv[:, 0, :] = 1.0 if half == 1 else 0.0
    c["kval"] = kv.astype(NPBF)
    c["ident"] = np.eye(128, dtype=np.float32).astype(NPBF)
    r = np.zeros((128, 128), np.float32)
    for mm in range(32):
        r[(mm + 16) % 32, mm] = 1.0
    c["rm"] = r.astype(NPBF)
    pos = (np.arange(CTX) - OWN + half * OWN).astype(np.float32)
    inv = (np.float32(500000.0) ** (-np.arange(0, 32, 2, dtype=np.float32) / np.float32(32))).astype(np.float32)
    ang = pos[None, :] * inv[:, None]
    cs, sn = np.cos(ang).astype(np.float32), np.sin(ang).astype(np.float32)
    c["ropeC"] = np.concatenate([cs, cs, np.ones((96, CTX), np.float32)], 0)
    c["ropeS"] = np.concatenate([-sn, sn, np.zeros((96, CTX), np.float32)], 0)
    return c


def prep_core(inp, b, half):
    f = lambda a: np.ascontiguousarray(np.asarray(a, dtype=np.float32))
    x = np.asarray(inp["x"])
    xcv = np.zeros((CTX, DM), np.float32)
    if half == 1:
        xcv[:] = x[b]
    else:
        xcv[OWN:] = x[b, :OWN]
    m = {"xc": xcv, "w_in": f(inp["w_in"][0]), "w_out": f(inp["w_out"][0]), "w_gate": f(inp["w_gate"][0]),
         "w_up": f(inp["w_up"][0]), "w_down": f(inp["w_down"][0]),
         "ck_w1": f(inp["ck_w1"][0]), "cv_w1": f(inp["cv_w1"][0]), "ck_w2": f(inp["ck_w2"][0]),
         "cv_w2": f(inp["cv_w2"][0]), "ck_peT": f(np.asarray(inp["ck_pe"][0]).T), "cv_peT": f(np.asarray(inp["cv_pe"][0]).T),
         "g_attn": f(inp["norm_attn"][0]).reshape(1, DM), "g_ffn": f(inp["norm_ffn"][0]).reshape(1, DM),
         "g_fin": f(inp["norm_final"]).reshape(1, DM),
         "g_outT": f(np.concatenate([np.asarray(inp["out_norm_a"][0]), np.asarray(inp["out_norm_b"][0])]).reshape(32, 128).T)}
    m.update(_consts(half))
    return m


def kernel(**inputs):
    nc = build_program()
    in_maps = [prep_core(inputs, c // 2, c % 2) for c in range(8)]
    res = run_bass_kernel_spmd(nc, in_maps, core_ids=list(range(8)))
    out = np.zeros((4, SEQ, DM), np.float32)
    for c in range(8):
        out[c // 2, (c % 2) * OWN:(c % 2 + 1) * OWN] = res.results[c]["y"]
    return out
```
